# Optimizing a Trainium2 kernel written in Bass

```python
import math
import jax, jax.numpy as jnp
from jax import lax
import numpy as np

D_MODEL = 2048
BATCH = 1
SEQ = 16384
DEPTH = 1

GRID_W = 64
ROPE_THETA = 10000.0
N_Q_HEADS = 8
N_KV_HEADS = 2
HEAD_DIM = 128
GQA_GROUP = N_Q_HEADS // N_KV_HEADS
BLOCK_Q = 128
ATTN_WIDTH = N_Q_HEADS * HEAD_DIM
KV_WIDTH = N_KV_HEADS * HEAD_DIM
SGU_GROUPS = 8
SGU_GROUP_DIM = 128
SGU_WIDTH = SGU_GROUPS * SGU_GROUP_DIM
SGU_CHUNK = 128
N_BRANCHES = 2
IN_WIDTH = ATTN_WIDTH + 2 * KV_WIDTH + 2 * SGU_WIDTH + N_BRANCHES * D_MODEL
SPLITS = (ATTN_WIDTH,
          ATTN_WIDTH + KV_WIDTH,
          ATTN_WIDTH + 2 * KV_WIDTH,
          ATTN_WIDTH + 2 * KV_WIDTH + SGU_WIDTH,
          ATTN_WIDTH + 2 * KV_WIDTH + 2 * SGU_WIDTH)
PEER_HEADS = 8
PEER_N_KEYS = 128
PEER_N_EXPERTS = PEER_N_KEYS * PEER_N_KEYS
PEER_QUERY_DIM = 256
PEER_HALF = PEER_QUERY_DIM // 2
PEER_TOPK = 16
PEER_BLOCK = 128
EPS = 1e-6

kernel_name = "hybrid_gqa_sgu_peer_block"


def rmsnorm(x, g):
    xf = x.astype(jnp.float32)
    y = xf * lax.rsqrt(jnp.mean(xf * xf, axis=-1, keepdims=True) + EPS)
    return (y * g.astype(jnp.float32)).astype(x.dtype)


def axial_rope_tables(seq_len):
    rows = seq_len // GRID_W
    row = jnp.repeat(jnp.arange(rows, dtype=jnp.float32), GRID_W)
    col = jnp.tile(jnp.arange(GRID_W, dtype=jnp.float32), rows)
    n_pairs = HEAD_DIM // 4
    inv_freq = ROPE_THETA ** (-jnp.arange(n_pairs, dtype=jnp.float32) / n_pairs)
    ang = jnp.concatenate([row[:, None] * inv_freq, col[:, None] * inv_freq], axis=-1)
    return jnp.cos(ang), jnp.sin(ang)


def apply_rope(x, cos, sin):
    B, S, H, D = x.shape
    xp = x.astype(jnp.float32).reshape(B, S, H, D // 2, 2)
    x1, x2 = xp[..., 0], xp[..., 1]
    c = cos[None, :, None, :]
    s = sin[None, :, None, :]
    out = jnp.stack([x1 * c - x2 * s, x1 * s + x2 * c], axis=-1)
    return out.reshape(B, S, H, D).astype(x.dtype)


def block_attention(q, k, v):
    B, S, _, hd = q.shape
    n_blk = S // BLOCK_Q
    scale = 1.0 / math.sqrt(HEAD_DIM)
    qb = jnp.moveaxis(q.reshape(B, n_blk, BLOCK_Q, N_KV_HEADS, GQA_GROUP, hd), 1, 0)

    def one_block(q_blk):
        s = jnp.einsum('bqkgd,bskd->bkgqs', q_blk, k).astype(jnp.float32) * scale
        p = jax.nn.softmax(s, axis=-1).astype(v.dtype)
        return jnp.einsum('bkgqs,bskd->bqkgd', p, v)

    o = lax.map(one_block, qb)
    return jnp.moveaxis(o, 0, 1).reshape(B, S, N_Q_HEADS * hd)


def spatial_gating(u, v, g_v, w_s, b_s):
    B, S, _ = u.shape
    n_chunk = S // SGU_CHUNK
    vn = rmsnorm(v, g_v).reshape(B, n_chunk, SGU_CHUNK, SGU_GROUPS, SGU_GROUP_DIM)
    mixed = jnp.einsum('gpq,bcqgd->bcpgd', w_s, vn) + b_s.T[None, None, :, :, None]
    return u * mixed.reshape(B, S, SGU_WIDTH)


def peer_ffn(xn, w_q, sub_k1, sub_k2, u_tab, v_tab):
    B, S, D = xn.shape
    q = (xn @ w_q).reshape(B, S, PEER_HEADS, PEER_QUERY_DIM)
    s1 = jnp.einsum('bshd,hnd->bshn', q[..., :PEER_HALF], sub_k1).astype(jnp.float32)
    s2 = jnp.einsum('bshd,hnd->bshn', q[..., PEER_HALF:], sub_k2).astype(jnp.float32)
    v1, i1 = lax.top_k(s1, PEER_TOPK)
    v2, i2 = lax.top_k(s2, PEER_TOPK)
    cand = (v1[..., :, None] + v2[..., None, :]).reshape(B, S, PEER_HEADS, PEER_TOPK * PEER_TOPK)
    cv, ci = lax.top_k(cand, PEER_TOPK)
    e1 = jnp.take_along_axis(i1, ci // PEER_TOPK, axis=-1)
    e2 = jnp.take_along_axis(i2, ci % PEER_TOPK, axis=-1)
    experts = (e1 * PEER_N_KEYS + e2).astype(jnp.int32)
    gates = jax.nn.softmax(cv, axis=-1).astype(xn.dtype)
    T = B * S
    n_blk = T // PEER_BLOCK
    hk = PEER_HEADS * PEER_TOPK
    xt = xn.reshape(n_blk, PEER_BLOCK, D)
    et = experts.reshape(n_blk, PEER_BLOCK, hk)
    gt = gates.reshape(n_blk, PEER_BLOCK, hk)

    def one_block(args):
        xb, eb, gb = args
        ue = jnp.take(u_tab, eb, axis=0)
        a = jax.nn.gelu(jnp.einsum('tkd,td->tk', ue, xb)) * gb
        ve = jnp.take(v_tab, eb, axis=0)
        return jnp.einsum('tk,tkd->td', a, ve)

    return lax.map(one_block, (xt, et, gt)).reshape(B, S, D)


def setup_inputs(seed: int = 0) -> dict:
    key = jax.random.key(seed)
    ks = jax.random.split(key, 20)
    f32 = jnp.float32
    nrm = lambda k, shape, scale: jax.random.normal(k, shape, f32) * scale
    L = DEPTH
    return {
        "x": nrm(ks[0], (BATCH, SEQ, D_MODEL), 1.0),
        "norm_mix": 1.0 + nrm(ks[1], (L, D_MODEL), 0.01),
        "w_in": nrm(ks[2], (L, D_MODEL, IN_WIDTH), D_MODEL ** -0.5),
        "b_gate": nrm(ks[3], (L, N_BRANCHES * D_MODEL), 0.01),
        "q_norm": 1.0 + nrm(ks[4], (L, HEAD_DIM), 0.01),
        "k_norm": 1.0 + nrm(ks[5], (L, HEAD_DIM), 0.01),
        "sgu_norm": 1.0 + nrm(ks[6], (L, SGU_WIDTH), 0.01),
        "w_sgu": nrm(ks[7], (L, SGU_GROUPS, SGU_CHUNK, SGU_CHUNK), SGU_CHUNK ** -0.5),
        "b_sgu": 1.0 + nrm(ks[8], (L, SGU_GROUPS, SGU_CHUNK), 0.01),
        "w_attn_proj": nrm(ks[9], (L, ATTN_WIDTH, D_MODEL), ATTN_WIDTH ** -0.5),
        "w_sgu_proj": nrm(ks[10], (L, SGU_WIDTH, D_MODEL), SGU_WIDTH ** -0.5),
        "w_out": nrm(ks[11], (L, D_MODEL, D_MODEL), D_MODEL ** -0.5),
        "norm_ffn": 1.0 + nrm(ks[12], (L, D_MODEL), 0.01),
        "w_peer_q": nrm(ks[13], (L, D_MODEL, PEER_HEADS * PEER_QUERY_DIM), D_MODEL ** -0.5),
        "peer_k1": nrm(ks[14], (L, PEER_HEADS, PEER_N_KEYS, PEER_HALF), PEER_HALF ** -0.5),
        "peer_k2": nrm(ks[15], (L, PEER_HEADS, PEER_N_KEYS, PEER_HALF), PEER_HALF ** -0.5),
        "peer_u": nrm(ks[16], (L, PEER_N_EXPERTS, D_MODEL), D_MODEL ** -0.5),
        "peer_v": nrm(ks[17], (L, PEER_N_EXPERTS, D_MODEL), PEER_HEADS ** -0.5),
    }


def reference(x, norm_mix, w_in, b_gate, q_norm, k_norm, sgu_norm, w_sgu, b_sgu,
              w_attn_proj, w_sgu_proj, w_out, norm_ffn, w_peer_q, peer_k1, peer_k2,
              peer_u, peer_v):
    B, S, _ = x.shape
    cos, sin = axial_rope_tables(S)
    h = x
    for l in range(DEPTH):
        hn = rmsnorm(h, norm_mix[l])
        z = hn @ w_in[l]
        q, k, v, su, sv, g = jnp.split(z, SPLITS, axis=-1)
        q = apply_rope(rmsnorm(q.reshape(B, S, N_Q_HEADS, HEAD_DIM), q_norm[l]), cos, sin)
        k = apply_rope(rmsnorm(k.reshape(B, S, N_KV_HEADS, HEAD_DIM), k_norm[l]), cos, sin)
        v = v.reshape(B, S, N_KV_HEADS, HEAD_DIM)
        attn = block_attention(q, k, v)
        sgu = spatial_gating(jax.nn.gelu(su), jax.nn.gelu(sv), sgu_norm[l], w_sgu[l], b_sgu[l])
        gates = jax.nn.sigmoid((g + b_gate[l]).astype(jnp.float32)).astype(h.dtype)
        g_attn, g_sgu = jnp.split(gates, N_BRANCHES, axis=-1)
        merged = g_attn * (attn @ w_attn_proj[l]) + g_sgu * (sgu @ w_sgu_proj[l])
        h = h + merged @ w_out[l]
        h = h + peer_ffn(rmsnorm(h, norm_ffn[l]), w_peer_q[l], peer_k1[l], peer_k2[l],
                         peer_u[l], peer_v[l])
    return h
```

```python
import numpy as np
from contextlib import ExitStack
import concourse.bass as bass
import concourse.mybir as mybir
from concourse.bass_utils import run_bass_kernel_spmd

F32 = mybir.dt.float32
BF16 = mybir.dt.bfloat16
U32 = mybir.dt.uint32
AF = mybir.ActivationFunctionType
ALU = mybir.AluOpType
AX = mybir.AxisListType

PE, ACT, DVE, POOL, SP = "tensor", "scalar", "vector", "gpsimd", "sync"
ENGINES = (PE, ACT, DVE, POOL, SP)
N_DMA_SEMS = 48

D = 2048
EPS = 1e-6
C_Q, C_K, C_V, C_SU, C_SV, C_GA, C_GG = 0, 1024, 1280, 1536, 2560, 3584, 5632
NEXP = 16384


class Buf:
    __slots__ = ("name", "last_write", "readers")

    def __init__(self, name=""):
        self.name = name
        self.last_write = None
        self.readers = []


class Op:
    __slots__ = ("eng", "fn", "deps", "is_dma", "signal", "sem", "val")

    def __init__(self, eng, fn, is_dma):
        self.eng = eng
        self.fn = fn
        self.deps = []
        self.is_dma = is_dma
        self.signal = False
        self.sem = None
        self.val = None


class Prog:
    def __init__(self):
        self.ops = {e: [] for e in ENGINES}
        self.all_ops = []
        self.pending = {e: None for e in ENGINES}
        self.dma_since = []

    def add(self, eng, fn, reads=(), writes=(), deps=(), dma=False):
        op = Op(eng, fn, dma)
        ds = []
        for b in reads:
            if b.last_write is not None:
                ds.append(b.last_write)
        for b in writes:
            if b.last_write is not None:
                ds.append(b.last_write)
            ds.extend(b.readers)
        ds.extend(d for d in deps if d is not None)
        if self.pending[eng] is not None:
            ds.extend(self.pending[eng])
            self.pending[eng] = None
        seen = set()
        for d in ds:
            if id(d) in seen:
                continue
            seen.add(id(d))
            if (not d.is_dma) and (not dma) and d.eng == eng and eng == PE:
                continue
            op.deps.append(d)
            d.signal = True
        for b in reads:
            b.readers.append(op)
        for b in writes:
            b.last_write = op
            b.readers = []
        self.ops[eng].append(op)
        self.all_ops.append(op)
        if dma:
            self.dma_since.append(op)
        return op

    def barrier(self):
        last = []
        for e in ENGINES:
            for o in reversed(self.ops[e]):
                if not o.is_dma:
                    last.append(o)
                    break
        last.extend(self.dma_since)
        self.dma_since = []
        for e in ENGINES:
            self.pending[e] = list(last)

    def prepare(self, eng_sems, dma_sems, final_ops):
        for o in final_ops:
            o.signal = True
        cnt = {e: 0 for e in ENGINES}
        dma_tot = [0] * len(dma_sems)
        dma_last = [None] * len(dma_sems)
        n_all = len(dma_sems)
        pools = {SP: (0, n_all // 2), ACT: (n_all // 2, n_all // 2 + n_all // 8),
                 POOL: (n_all // 2 + n_all // 8, n_all)}
        rrq = {SP: 0, ACT: 0, POOL: 0}
        for op in self.all_ops:
            if op.is_dma:
                lo, hi = pools[op.eng]
                k = lo + rrq[op.eng] % (hi - lo)
                rrq[op.eng] += 1
                if dma_last[k] is not None:
                    op.deps.append(dma_last[k])
                dma_tot[k] += 16
                op.sem = dma_sems[k]
                op.val = dma_tot[k]
                dma_last[k] = op
            elif op.signal:
                cnt[op.eng] += 1
                op.sem = eng_sems[op.eng]
                op.val = cnt[op.eng]
        self.final_ops = final_ops

    def emit(self, eng, e):
        waited = {}
        for op in self.ops[eng]:
            need = {}
            for d in op.deps:
                key = id(d.sem)
                if key not in need or need[key][1] < d.val:
                    need[key] = (d.sem, d.val)
            for key, (sem, val) in need.items():
                if waited.get(key, 0) >= val:
                    continue
                e.wait_ge(sem, val)
                waited[key] = val
            ins = op.fn(e)
            if op.is_dma:
                ins.then_inc(op.sem, 16)
            elif op.signal:
                ins.then_inc(op.sem, 1)
        if eng == SP:
            for o in self.final_ops:
                key = id(o.sem)
                if waited.get(key, 0) < o.val:
                    e.wait_ge(o.sem, o.val)
                    waited[key] = o.val


class Ring:
    def __init__(self, alloc, name, shape, dt, n):
        self.tiles = [alloc("%s%d" % (name, i), shape, dt) for i in range(n)]
        self.bufs = [Buf("%s%d" % (name, i)) for i in range(n)]
        self.i = 0

    def next(self):
        k = self.i % len(self.tiles)
        self.i += 1
        return self.tiles[k], self.bufs[k]


def cap(ap, dims):
    return bass.AP(ap.tensor, ap.offset, [list(ap.ap[0])] + [[s, n] for s, n in dims])


def build(S, NC, dbg=False, phases="ABCE"):
    NT_ALL = S // 128
    TOK = S // NC
    NT_OWN = TOK // 128
    TC = 512
    NCH = TOK // TC
    nc = bass.Bass("TRN2", target_bir_lowering=False)
    P = Prog()

    def din(name, shape, dt=F32):
        return nc.dram_tensor(name, list(shape), dt, kind="ExternalInput").ap()

    x_all = din("x", [S, D])
    x_own = din("x_own", [TOK, D])
    cs_all = din("cs", [S, 128])
    cs_own = din("cs_own", [TOK, 128])
    norm_mix = din("norm_mix", [16, 128])
    w_in = din("w_in", [D, 7680])
    b_gate = din("b_gate", [32, 128])
    q_norm = din("q_norm", [1, 128])
    k_norm = din("k_norm", [1, 128])
    sgu_norm = din("sgu_norm", [1, 1024])
    w_sgu = din("w_sgu", [8, 128, 128])
    b_sgu = din("b_sgu", [1, 1024])
    w_ap = din("w_attn_proj", [1024, D])
    w_gp = din("w_sgu_proj", [1024, D])
    w_out = din("w_out", [D, D])
    norm_ffn = din("norm_ffn", [16, 128])
    w_pq = din("w_peer_q", [D, D])
    pk1 = din("peer_k1", [8, 128, 128])
    pk2 = din("peer_k2", [8, 128, 128])
    peer_u = din("peer_u", [NEXP, D])
    peer_v = din("peer_v", [NEXP, D])
    out = nc.dram_tensor("out", [TOK, D], F32, kind="ExternalOutput").ap()
    skind = "ExternalOutput" if dbg else "Internal"
    KT = nc.dram_tensor("KT", [2, 128, S], BF16, kind=skind).ap()
    VD = nc.dram_tensor("VD", [2, S, 128], BF16, kind=skind).ap()
    H2 = nc.dram_tensor("H2", [TOK, D], F32, kind=skind).ap()
    WINb = nc.dram_tensor("WINb", [D, 7680], BF16, kind="Internal").ap()
    WAPb = nc.dram_tensor("WAPb", [1024, D], BF16, kind="Internal").ap()
    WGPb = nc.dram_tensor("WGPb", [1024, D], BF16, kind="Internal").ap()
    WOUTb = nc.dram_tensor("WOUTb", [D, D], BF16, kind="Internal").ap()
    WPQb = nc.dram_tensor("WPQb", [D, D], BF16, kind="Internal").ap()
    UB16 = nc.dram_tensor("UB16", [NEXP, D], BF16, kind="Internal").ap()
    VB16 = nc.dram_tensor("VB16", [NEXP, D], BF16, kind="Internal").ap()
    UT16 = nc.dram_tensor("UT16", [128, 128, 16 * 128], BF16, kind="Internal").ap()
    UTB = [Buf() for _ in range(128)]
    WcB = Buf("wcast")
    UcB = [Buf() for _ in range(NEXP // 512)]
    VcB = [Buf() for _ in range(NEXP // 512)]
    if dbg:
        QAd = nc.dram_tensor("QAd", [128, 8, TOK], BF16, kind="ExternalOutput").ap()
        ATd = nc.dram_tensor("ATd", [128, 8, TOK], BF16, kind="ExternalOutput").ap()
        SGd = nc.dram_tensor("SGd", [128, 8, TOK], BF16, kind="ExternalOutput").ap()
        MTd = nc.dram_tensor("MTd", [128, 16, TOK], BF16, kind="ExternalOutput").ap()
        RTd = nc.dram_tensor("RTd", [3, 128, TOK], F32, kind="ExternalOutput").ap()
    KTB = [[Buf() for _ in range(NT_ALL)] for _ in range(2)]
    VDB = [[Buf() for _ in range(NT_ALL)] for _ in range(2)]
    H2B = [Buf() for _ in range(NT_OWN)]
    final_ops = []

    with ExitStack() as es0:
        def sb0(name, shape, dt):
            return es0.enter_context(nc.sbuf_tensor(name, list(shape), dt))

        eng_sems = {e: es0.enter_context(nc.semaphore("s_" + e)) for e in ENGINES}
        dma_sems = [es0.enter_context(nc.semaphore("d%d" % i)) for i in range(N_DMA_SEMS)]

        def dma(o, i, reads=(), writes=(), q=SP):
            return P.add(q, lambda e: e.dma_start(out=o, in_=i), reads, writes, dma=True)

        def mm(o, lhsT, rhs, start, stop, reads=(), writes=()):
            return P.add(PE, lambda e: e.matmul(o, lhsT=lhsT, rhs=rhs, start=start, stop=stop), reads, writes)

        def tr(o, i, ident, reads=(), writes=()):
            return P.add(PE, lambda e: e.transpose(out=o, in_=i, identity=ident), reads, writes)

        def act(o, i, func, reads=(), writes=(), **kw):
            return P.add(ACT, lambda e: e.activation(out=o, in_=i, func=func, **kw), reads, writes)

        def tt(o, a, b, op, reads=(), writes=(), eng=DVE):
            return P.add(eng, lambda e: e.tensor_tensor(out=o, in0=a, in1=b, op=op), reads, writes)

        def ts(o, a, s1, s2, op0, op1=None, reads=(), writes=(), eng=DVE):
            if op1 is None:
                return P.add(eng, lambda e: e.tensor_scalar(out=o, in0=a, scalar1=s1, scalar2=None, op0=op0), reads, writes)
            return P.add(eng, lambda e: e.tensor_scalar(out=o, in0=a, scalar1=s1, scalar2=s2, op0=op0, op1=op1), reads, writes)

        def cp(o, i, reads=(), writes=(), eng=DVE):
            return P.add(eng, lambda e: e.tensor_copy(out=o, in_=i), reads, writes)

        def red(o, i, reads=(), writes=()):
            return P.add(DVE, lambda e: e.tensor_reduce(out=o, in_=i, axis=AX.X, op=ALU.add), reads, writes)

        def rcp(o, i, reads=(), writes=()):
            return P.add(DVE, lambda e: e.reciprocal(out=o, in_=i), reads, writes)

        ident_f = sb0("ident_f", [128, 128], F32)
        ident_b = sb0("ident_b", [128, 128], BF16)
        iota_b = sb0("iota_b", [128, 128], BF16)
        iota_f = sb0("iota_f", [128, 128], F32)
        iota_p = sb0("iota_p", [128, 1], F32)
        gmixT = sb0("gmixT", [128, 16], F32)
        gffnT = sb0("gffnT", [128, 16], F32)
        CB = Buf("consts")
        P.add(POOL, lambda e: e.iota(iota_p[:], pattern=[[0, 1]], base=0, channel_multiplier=1,
                                     allow_small_or_imprecise_dtypes=True), writes=[CB])
        P.add(POOL, lambda e: e.iota(iota_f[:], pattern=[[1, 128]], base=0, channel_multiplier=0,
                                     allow_small_or_imprecise_dtypes=True), writes=[CB])
        ts(ident_f[:], iota_f[:], iota_p[:, 0:1], None, ALU.is_equal, reads=[CB], writes=[CB])
        cp(ident_b[:], ident_f[:], reads=[CB], writes=[CB])
        cp(iota_b[:], iota_f[:], reads=[CB], writes=[CB])

        esQA = ExitStack()
        QA = esQA.enter_context(nc.sbuf_tensor("QA", [128, 8, TOK], BF16))
        QAB = [[Buf() for _ in range(NT_OWN)] for _ in range(2)]

        def load_colvec(dst, src, rows, es):
            tmp = es.enter_context(nc.sbuf_tensor("cv_tmp_%s" % dst.name, [rows, 128], F32))
            ptmp = es.enter_context(nc.psum_tensor("cv_ps_%s" % dst.name, [128, rows], F32))
            b = Buf()
            dma(tmp[:], src, writes=[b])
            tr(ptmp[:], tmp[:], ident_f[0:rows, 0:rows], reads=[b, CB], writes=[b])
            cp(dst[:], ptmp[:], reads=[b], writes=[CB])

        with ExitStack() as es:
            load_colvec(gmixT, norm_mix, 16, es)
            load_colvec(gffnT, norm_ffn, 16, es)
        P.barrier()

        def norm_T(src, gT, dst, dstB, R):
            xt, xtB = R["xt"].next()
            dma(xt[:], src, writes=[xtB])
            return norm_T_sb(xt[:], xtB, gT, dst, dstB, R)

        def norm_T_sb(xt, xtB, gT, dst, dstB, R, defer=False, dstB2=None):
            st, stB = R["st"].next()
            xs, xsB = R["xs"].next()
            pT, pTB = R["pT"].next()
            act(xs[:], xt, AF.Square, reads=[xtB], writes=[xsB, stB], accum_out=st[:, 0:1])
            act(st[:, 1:2], st[:, 0:1], AF.Sqrt, reads=[stB], writes=[stB], scale=1.0 / D, bias=EPS)
            rcp(st[:, 2:3], st[:, 1:2], reads=[stB], writes=[stB])
            if defer:
                act(xs[:], xt, AF.Copy, reads=[xtB], writes=[xsB])
            else:
                act(xs[:], xt, AF.Copy, reads=[xtB, stB], writes=[xsB], scale=st[:, 2:3])
            for dt in range(16):
                tr(pT[:, dt, :], xs[:, dt * 128:(dt + 1) * 128], ident_b[:], reads=[xsB, CB], writes=[pTB])
            if dstB2 is None:
                tt(dst, pT[:], cap(gT[:], [(1, 16), (0, 128)]), ALU.mult, reads=[pTB, CB], writes=[dstB])
            else:
                tt(dst[:, 0:8, :], pT[:, 0:8, :], cap(gT[:, 0:8], [(1, 8), (0, 128)]), ALU.mult,
                   reads=[pTB, CB], writes=[dstB])
                tt(dst[:, 8:16, :], pT[:, 8:16, :], cap(gT[:, 8:16], [(1, 8), (0, 128)]), ALU.mult,
                   reads=[pTB, CB], writes=[dstB2])
            return st, stB

        def qk_norm_rope(src, srcB, nh, gain, cst, cstB, kr, krB, R):
            W = nh * 128
            sq, kn, t1, t2, sm = R["sq"], R["kn"], R["t1"], R["t2"], R["sm"]
            TB = R["ropeB"]
            src3 = src.rearrange("p (h d) -> p h d", d=128)
            act(sq[:, 0:W], src, AF.Square, reads=[srcB], writes=[TB])
            red(sm[:, 0:nh], sq[:, 0:W].rearrange("p (h d) -> p h d", d=128), reads=[TB], writes=[TB])
            act(sm[:, 16:16 + nh], sm[:, 0:nh], AF.Sqrt, reads=[TB], writes=[TB], scale=1.0 / 128, bias=EPS)
            rcp(sm[:, 32:32 + nh], sm[:, 16:16 + nh], reads=[TB], writes=[TB])
            kn3 = kn[:, 0:W].rearrange("p (h d) -> p h d", d=128)
            tt(kn3, src3, cap(sm[:, 32:32 + nh], [(1, nh), (0, 128)]), ALU.mult, reads=[srcB, TB], writes=[TB])
            tt(kn3, kn3, gain, ALU.mult, reads=[TB, CB], writes=[TB])
            x1 = kn[:, 0:W:2].rearrange("p (h j) -> p h j", j=64)
            x2 = kn[:, 1:W:2].rearrange("p (h j) -> p h j", j=64)
            c = cap(cst[:, 0:64], [(0, nh), (1, 64)])
            s = cap(cst[:, 64:128], [(0, nh), (1, 64)])
            a1 = t1[:, 0:nh * 64].rearrange("p (h j) -> p h j", j=64)
            a2 = t2[:, 0:nh * 64].rearrange("p (h j) -> p h j", j=64)
            tt(a1, x1, c, ALU.mult, reads=[TB, cstB], writes=[TB])
            tt(a2, x2, s, ALU.mult, reads=[TB, cstB], writes=[TB])
            tt(kr[:, :, 0:128:2], a1, a2, ALU.subtract, reads=[TB], writes=[krB])
            TB2 = R["ropeB2"]
            b1 = R["t3"][:, 0:nh * 64].rearrange("p (h j) -> p h j", j=64)
            b2 = R["t4"][:, 0:nh * 64].rearrange("p (h j) -> p h j", j=64)
            tt(b1, x1, s, ALU.mult, reads=[TB, cstB], writes=[TB2])
            tt(b2, x2, c, ALU.mult, reads=[TB, cstB], writes=[TB2])
            tt(kr[:, :, 1:128:2], b1, b2, ALU.add, reads=[TB2], writes=[krB])

        def wload(dst, src, writes, nsplit=4):
            k = dst.shape[1]
            step = max(1, k // nsplit)
            ops = []
            for a in range(0, k, step):
                ops.append(dma(dst[:, a:a + step, :], src[:, a:a + step, :], writes=writes, q=POOL))
            return ops

        def emit_precasts(deps):
            def precast(dst, src, rows, cols, writes):
                k = 1920 if cols == 7680 else cols
                sv = src.rearrange("r (c k) -> (r c) k", k=k)
                dv = dst.rearrange("r (c k) -> (r c) k", k=k)
                n = rows * (cols // k)
                for a in range(0, n, 512):
                    P.add(POOL, lambda e, o=dv[a:a + 512, :], i=sv[a:a + 512, :]: e.dma_start(out=o, in_=i),
                          (), writes(a), deps=deps, dma=True)
            if "C" in phases:
                precast(WINb, w_in, D, 7680, lambda a: [WcB])
                precast(WAPb, w_ap, 1024, D, lambda a: [WcB])
                precast(WGPb, w_gp, 1024, D, lambda a: [WcB])
                precast(WOUTb, w_out, D, D, lambda a: [WcB])
            if "E" in phases:
                precast(WPQb, w_pq, D, D, lambda a: [WcB])
                precast(UB16, peer_u, NEXP, D, lambda a: [UcB[a // 512]])
                precast(VB16, peer_v, NEXP, D, lambda a: [VcB[a // 512]])

        w_in_v = w_in.rearrange("(dt p) n -> p dt n", p=128)

        if "A" in phases:
            with ExitStack() as es:
                def sb(name, shape, dt):
                    return es.enter_context(nc.sbuf_tensor("A_" + name, list(shape), dt))

                def ps(name, shape, dt):
                    return es.enter_context(nc.psum_tensor("A_" + name, list(shape), dt))

                R = {
                    "xt": Ring(sb, "xt", [128, D], F32, 3),
                    "st": Ring(sb, "st", [128, 4], F32, 4),
                    "xs": Ring(sb, "xs", [128, D], BF16, 2),
                    "pT": Ring(ps, "pT", [128, 16, 128], BF16, 1),
                    "sq": sb("sq", [128, 1024], F32), "kn": sb("kn", [128, 1024], F32),
                    "t1": sb("t1", [128, 512], F32), "t2": sb("t2", [128, 512], F32),
                    "t3": sb("t3", [128, 512], F32), "t4": sb("t4", [128, 512], F32), "ropeB2": Buf(),
                    "sm": sb("sm", [128, 48], F32), "ropeB": Buf(),
                }
                xsT_r = Ring(sb, "xsT", [128, 16, 128], BF16, 2)
                xsT_hiB = [Buf(), Buf()]
                cs_r = Ring(sb, "cst", [128, 128], F32, 4)
                wkv = sb("wkv", [128, 16, 512], BF16)
                wq = sb("wq", [128, 16, 1024], BF16)
                gqk = sb("gqk", [128, 256], F32)
                WB = Buf("wA")
                wload(wkv[:], w_in_v[:, :, C_K:C_K + 512], [WB])
                wload(wq[:], w_in_v[:, :, C_Q:C_Q + 1024], [WB], nsplit=8)
                if "B" not in phases:
                    emit_precasts([])
                dma(gqk[:, 0:128], q_norm[0, :].partition_broadcast(128), writes=[CB])
                dma(gqk[:, 128:256], k_norm[0, :].partition_broadcast(128), writes=[CB])
                pbig = Ring(ps, "pbig", [128, 1024], F32, 2)
                pKT = Ring(ps, "pKT", [128, 8, 128], BF16, 2)
                kr_r = Ring(sb, "kr", [128, 8, 128], BF16, 2)
                kvs_r = Ring(sb, "kvs", [128, 1024], F32, 2)
                vb_r = Ring(sb, "vb", [128, 2, 128], BF16, 3)
                kT_r = Ring(sb, "kTs", [128, 2, 128], BF16, 3)

                def a_stage0(xsrc, cssrc, i):
                    cst, cstB = cs_r.next()
                    dma(cst[:], cssrc[i * 128:(i + 1) * 128, :], writes=[cstB])
                    xt, xtB = R["xt"].next()
                    dma(xt[:], xsrc[i * 128:(i + 1) * 128, :], writes=[xtB])
                    return (cst, cstB, xt, xtB)

                def a1_stage1(i, ld):
                    hiB = xsT_hiB[xsT_r.i % 2]
                    xsT, xsTB = xsT_r.next()
                    cst, cstB, xt, xtB = ld
                    st, stB = norm_T_sb(xt[:], xtB, gmixT, xsT[:], xsTB, R, defer=True, dstB2=hiB)
                    pk, pkB = pbig.next()
                    for dt in range(16):
                        mm(pk[:, 0:512], xsT[:, dt, :], wkv[:, dt, :], dt == 0, dt == 15,
                           reads=[xsTB if dt < 8 else hiB, WB], writes=[pkB])
                    return (i, pk, pkB, cst, cstB, st, stB)

                def a1_stage2(stt):
                    i, pk, pkB, cst, cstB, st, stB = stt
                    kvs, kvsB = kvs_r.next()
                    ts(kvs[:, 0:512], pk[:, 0:512], st[:, 2:3], None, ALU.mult, reads=[pkB, stB], writes=[kvsB])
                    kr, krB = kr_r.next()
                    qk_norm_rope(kvs[:, 0:256], kvsB, 2, cap(gqk[:, 128:256], [(0, 2), (1, 128)]), cst, cstB,
                                 kr[:, 0:2, :], krB, R)
                    vb, vbB = vb_r.next()
                    cp(vb[:].rearrange("p h d -> p (h d)"), kvs[:, 256:512], reads=[kvsB], writes=[vbB], eng=POOL)
                    pt_, ptB = pKT.next()
                    for h in range(2):
                        tr(pt_[:, h, :], kr[:, h, :], ident_b[:], reads=[krB, CB], writes=[ptB])
                    def late():
                        kTs, kTB = kT_r.next()
                        cp(kTs[:], pt_[:, 0:2, :], reads=[ptB], writes=[kTB])
                        dma(KT[:, :, i * 128:(i + 1) * 128].rearrange("h d t -> d h t"), kTs[:], reads=[kTB],
                            writes=[KTB[0][i], KTB[1][i]])
                        dma(VD[:, i * 128:(i + 1) * 128, :].rearrange("h t d -> t h d"), vb[:], reads=[vbB],
                            writes=[VDB[0][i], VDB[1][i]])
                    return late

                def a2_stage1(i, ld):
                    hiB = xsT_hiB[xsT_r.i % 2]
                    xsT, xsTB = xsT_r.next()
                    cst, cstB, xt, xtB = ld
                    st, stB = norm_T_sb(xt[:], xtB, gmixT, xsT[:], xsTB, R, defer=True, dstB2=hiB)
                    pq, pqB = pbig.next()
                    for half in range(2):
                        for dt in range(16):
                            mm(pq[:, half * 512:(half + 1) * 512], xsT[:, dt, :], wq[:, dt, half * 512:(half + 1) * 512],
                               dt == 0, dt == 15, reads=[xsTB if dt < 8 else hiB, WB], writes=[pqB])
                    return (i, pq, pqB, cst, cstB, st, stB)

                def a2_stage2(stt):
                    i, pq, pqB, cst, cstB, st, stB = stt
                    kvs, kvsB = kvs_r.next()
                    ts(kvs[:], pq[:], st[:, 2:3], None, ALU.mult, reads=[pqB, stB], writes=[kvsB])
                    kr, krB = kr_r.next()
                    qk_norm_rope(kvs[:], kvsB, 8, cap(gqk[:, 0:128], [(0, 8), (1, 128)]), cst, cstB, kr[:], krB, R)
                    pt_, ptB = pKT.next()
                    for h in range(8):
                        tr(pt_[:, h, :], kr[:, h, :], ident_b[:], reads=[krB, CB], writes=[ptB])
                    def late():
                        cp(QA[:, :, i * 128:(i + 1) * 128], pt_[:], reads=[ptB], writes=[QAB[0][i], QAB[1][i]])
                    return late

                work = [(a1_stage1, a1_stage2, i, x_all, cs_all) for i in range(NT_ALL)] + \
                       [(a2_stage1, a2_stage2, i, x_own, cs_own) for i in range(NT_OWN)]
                NW = len(work)
                lds = {}
                for w in range(min(2, NW)):
                    lds[w] = a_stage0(work[w][3], work[w][4], work[w][2])
                cur = work[0][0](work[0][2], lds.pop(0))
                late_prev = None
                for w in range(NW):
                    if w + 2 < NW:
                        lds[w + 2] = a_stage0(work[w + 2][3], work[w + 2][4], work[w + 2][2])
                    nxt = work[w + 1][0](work[w + 1][2], lds.pop(w + 1)) if w + 1 < NW else None
                    if late_prev is not None:
                        late_prev()
                    late_prev = work[w][1](cur)
                    cur = nxt
                late_prev()
                if dbg:
                    final_ops.append(dma(QAd, QA[:], reads=[b for r in QAB for b in r]))
            P.barrier()

        if "B" in phases:
            with ExitStack() as es:
                def sb(name, shape, dt):
                    return es.enter_context(nc.sbuf_tensor("B_" + name, list(shape), dt))

                def ps(name, shape, dt):
                    return es.enter_context(nc.psum_tensor("B_" + name, list(shape), dt))

                KTs2 = [sb("KTs%d" % h, [128, S], BF16) for h in range(2)]
                V12 = [sb("V1%d" % h, [128, NT_ALL, 130], BF16) for h in range(2)]
                NSEG = 8 if NT_ALL >= 8 else (4 if NT_ALL >= 4 else 1)
                SEG = NT_ALL // NSEG
                KB2 = [[Buf() for _ in range(NSEG)] for h in range(2)]
                VB2 = [[Buf() for _ in range(NSEG)] for h in range(2)]
                pS = Ring(ps, "pS", [128, 512], F32, 3)
                pO = [ps("pO%d" % g, [128, 512], F32) for g in range(4)]
                pOB = [Buf() for _ in range(4)]
                pOT = Ring(ps, "pOT", [128, 4, 128], BF16, 1)
                pt_r = Ring(sb, "pt", [128, 512], BF16, 4)
                ob_r = Ring(sb, "ob", [128, 128], BF16, 4)
                rc_r = Ring(sb, "rc", [128, 1], F32, 4)
                P.add(POOL, lambda e: e.memset(V12[0][:], 1.0), writes=VB2[0])
                P.add(POOL, lambda e: e.memset(V12[1][:], 1.0), writes=VB2[1])
                kv_loads = []
                for kvh in range(2):
                    for sg in range(NSEG):
                        a, b = sg * SEG, (sg + 1) * SEG
                        kv_loads.append(dma(KTs2[kvh][:, a * 128:b * 128], KT[kvh, :, a * 128:b * 128],
                                            reads=KTB[kvh][a:b], writes=[KB2[kvh][sg]]))
                        kv_loads.append(dma(V12[kvh][:, a:b, 0:128],
                                            VD[kvh, a * 128:b * 128, :].rearrange("(n p) d -> p n d", p=128),
                                            reads=VDB[kvh][a:b], writes=[VB2[kvh][sg]]))
                emit_precasts(kv_loads)
                scale = 1.0 / float(np.sqrt(128.0))
                for kvh in range(2):
                    KTs, V1, KB, VB = KTs2[kvh], V12[kvh], KB2[kvh], VB2[kvh]
                    steps = [(qt, st) for qt in range(NT_OWN) for st in range(NT_ALL)]
                    LA = 2
                    pend = []

                    def emit_qk(idx):
                        qt, st = steps[idx]
                        qsl_ = QA[:, 4 * kvh:4 * kvh + 4, qt * 128:(qt + 1) * 128]
                        p_s, p_sB = pS.next()
                        mm(p_s[:], KTs[:, st * 128:(st + 1) * 128], qsl_, True, True,
                           reads=[KB[st // SEG], QAB[kvh][qt]], writes=[p_sB])
                        pend.append((p_s, p_sB))

                    for idx in range(min(LA, len(steps))):
                        emit_qk(idx)
                    for idx, (qt, st) in enumerate(steps):
                        if idx + LA < len(steps):
                            emit_qk(idx + LA)
                        qB = QAB[kvh][qt]
                        qsl = QA[:, 4 * kvh:4 * kvh + 4, qt * 128:(qt + 1) * 128]
                        sg = st // SEG
                        p_s, p_sB = pend.pop(0)
                        pt, ptB = pt_r.next()
                        act(pt[:], p_s[:], AF.Exp, reads=[p_sB], writes=[ptB], scale=scale)
                        for g in range(4):
                            mm(pO[g][:, 0:129], pt[:, g * 128:(g + 1) * 128], V1[:, st, 0:129], st == 0,
                               st == NT_ALL - 1, reads=[ptB, VB[sg]], writes=[pOB[g]])
                        if st == NT_ALL - 1:
                            po_t, po_tB = pOT.next()
                            for g in range(4):
                                rc, rcB = rc_r.next()
                                ob, obB = ob_r.next()
                                rcp(rc[:], pO[g][:, 128:129], reads=[pOB[g]], writes=[rcB])
                                ts(ob[:], pO[g][:, 0:128], rc[:, 0:1], None, ALU.mult, reads=[pOB[g], rcB], writes=[obB])
                                tr(po_t[:, g, :], ob[:], ident_b[:], reads=[obB, CB], writes=[po_tB])
                            act(qsl, po_t[:], AF.Copy, reads=[po_tB], writes=[qB])
                if dbg:
                    final_ops.append(dma(ATd, QA[:], reads=[b for r in QAB for b in r]))
            P.barrier()

        if "C" in phases:
            with ExitStack() as es:
                def sb(name, shape, dt):
                    return es.enter_context(nc.sbuf_tensor("C_" + name, list(shape), dt))

                def ps(name, shape, dt):
                    return es.enter_context(nc.psum_tensor("C_" + name, list(shape), dt))

                R = {
                    "xt": Ring(sb, "xt", [128, D], F32, 2),
                    "st": Ring(sb, "st", [128, 4], F32, 2),
                    "xs": Ring(sb, "xs", [128, D], BF16, 2),
                    "pT": Ring(ps, "pT", [128, 16, 128], BF16, 1),
                    "junk": sb("junk", [128, 1024], BF16), "junkB": Buf(),
                }
                wsT = sb("wsT", [128, 8, 128], BF16)
                bs_bc = sb("bs_bc", [128, 8, 128], F32)
                gsg = sb("gsg", [128, 1024], F32)
                bgT = sb("bgT", [128, 32], F32)
                with ExitStack() as es2:
                    load_colvec(bgT, b_gate, 32, es2)
                    wtmp = es2.enter_context(nc.sbuf_tensor("wtmp", [128, 8, 128], F32))
                    pw = es2.enter_context(nc.psum_tensor("pw", [128, 8, 128], F32))
                    b = Buf()
                    dma(wtmp[:], w_sgu.rearrange("g p q -> p g q"), writes=[b])
                    for g in range(8):
                        tr(pw[:, g, :], wtmp[:, g, :], ident_f[:], reads=[b, CB], writes=[b])
                    cp(wsT[:], pw[:], reads=[b], writes=[CB])
                    dma(bs_bc[:].rearrange("p g q -> p (g q)"), b_sgu[0, :].partition_broadcast(128), writes=[CB])
                    dma(gsg[:], sgu_norm[0, :].partition_broadcast(128), writes=[CB])
                    P.barrier()
                xsTc = sb("xsTc", [128, 16, TC], BF16)
                xsTcB = [Buf() for _ in range(TC // 128)]
                sguT = sb("sguT", [128, 8, TC], BF16)
                sguB = Buf()
                mT = sb("mT", [128, 16, TC], BF16)
                mTB = [Buf() for _ in range(16)]
                WBK = 256
                NJ = WBK // 128
                wblk = Ring(sb, "wblk", [128, 16, WBK], BF16, 4)
                wblk2 = Ring(sb, "wblk2", [128, 8, WBK], BF16, 4)
                gv = sb("gv", [128, 1024], F32)
                gvB = Buf()
                vn_r = Ring(sb, "vn", [128, 1024], BF16, 2)
                tmpm = sb("tmpm", [128, 8, 128], F32)
                tmpmB = Buf()
                sig_r = Ring(sb, "sig", [128, 512], F32, 4)
                m1_r = Ring(sb, "m1", [128, 512], F32, 4)
                xr_r = Ring(sb, "xr", [128, WBK], F32, 3)
                h2_r = Ring(sb, "h2t", [128, WBK], F32, 3)
                pbk = [ps("pbk%d" % k, [128, 512], F32) for k in range(6)]
                pbB = [Buf() for _ in range(6)]
                pbi = [0]

                def nextbank():
                    k = pbi[0] % 6
                    pbi[0] += 1
                    return pbk[k], pbB[k]

                win_v = WINb.rearrange("(dt p) n -> p dt n", p=128)
                wap_v = WAPb.rearrange("(kt p) n -> p kt n", p=128)
                wgp_v = WGPb.rearrange("(kt p) n -> p kt n", p=128)
                wout_v = WOUTb.rearrange("(kt p) n -> p kt n", p=128)

                def wl(ring, src):
                    wb, wbB = ring.next()
                    dma(wb[:], src, reads=[WcB], writes=[wbB], q=POOL)
                    return wb, wbB

                NTC = TC // 128
                for c in range(NCH):
                    for tl in range(NTC):
                        row = c * TC + tl * 128
                        norm_T(x_own[row:row + 128, :], gmixT, xsTc[:, :, tl * 128:(tl + 1) * 128], xsTcB[tl], R)
                    for blk in range(1024 // WBK):
                        wb, wbB = wl(wblk, win_v[:, :, C_SU + blk * WBK:C_SU + (blk + 1) * WBK])
                        for j in range(NJ):
                            f = blk * NJ + j
                            pb, pbB_ = nextbank()
                            for dt in range(16):
                                mm(pb[:], wb[:, dt, j * 128:(j + 1) * 128], xsTc[:, dt, :], dt == 0, dt == 15,
                                   reads=[wbB] + xsTcB, writes=[pbB_])
                            act(sguT[:, f, :], pb[:], AF.Gelu_apprx_tanh, reads=[pbB_], writes=[sguB])
                    wsv = [wl(wblk, win_v[:, :, C_SV + blk * WBK:C_SV + (blk + 1) * WBK]) for blk in range(1024 // WBK)]
                    for tl in range(NTC):
                        banks = [nextbank(), nextbank()]
                        for blk in range(1024 // WBK):
                            wb, wbB = wsv[blk]
                            pb, pbB_ = banks[(blk * WBK) // 512]
                            co = (blk * WBK) % 512
                            for dt in range(16):
                                mm(pb[:, co:co + WBK], xsTc[:, dt, tl * 128:(tl + 1) * 128], wb[:, dt, :], dt == 0, dt == 15,
                                   reads=[wbB, xsTcB[tl]], writes=[pbB_])
                        st, stB = R["st"].next()
                        for blk in range(2):
                            act(gv[:, blk * 512:(blk + 1) * 512], banks[blk][0][:], AF.Gelu_apprx_tanh,
                                reads=[banks[blk][1]], writes=[gvB])
                        act(R["junk"][:], gv[:], AF.Square, reads=[gvB], writes=[R["junkB"], stB],
                            accum_out=st[:, 0:1])
                        act(st[:, 1:2], st[:, 0:1], AF.Sqrt, reads=[stB], writes=[stB], scale=1.0 / 1024, bias=EPS)
                        rcp(st[:, 2:3], st[:, 1:2], reads=[stB], writes=[stB])
                        vn, vnB = vn_r.next()
                        P.add(DVE, lambda e, vn=vn, st=st: e.scalar_tensor_tensor(
                            out=vn[:], in0=gv[:], scalar=st[:, 2:3], in1=gsg[:], op0=ALU.mult, op1=ALU.mult),
                            reads=[gvB, stB, CB], writes=[vnB])
                        pm0, pm0B = nextbank()
                        pm1, pm1B = nextbank()
                        for g in range(8):
                            pm, pmB = (pm0, pm0B) if g < 4 else (pm1, pm1B)
                            mm(pm[:, (g % 4) * 128:(g % 4 + 1) * 128], vn[:, g * 128:(g + 1) * 128], wsT[:, g, :],
                               True, True, reads=[vnB, CB], writes=[pmB])
                        for hh, (pm, pmB) in enumerate(((pm0, pm0B), (pm1, pm1B))):
                            tt(tmpm[:, hh * 4:(hh + 1) * 4, :], pm[:].rearrange("p (g q) -> p g q", q=128),
                               bs_bc[:, hh * 4:(hh + 1) * 4, :], ALU.add, reads=[pmB, CB], writes=[tmpmB])
                        tt(sguT[:, :, tl * 128:(tl + 1) * 128], sguT[:, :, tl * 128:(tl + 1) * 128], tmpm[:], ALU.mult,
                           reads=[tmpmB, sguB], writes=[sguB])
                    if dbg:
                        final_ops.append(dma(SGd[:, :, c * TC:(c + 1) * TC], sguT[:], reads=[sguB]))
                    for blk in range(D // WBK):
                        wga, wgaB = wl(wblk, win_v[:, :, C_GA + blk * WBK:C_GA + (blk + 1) * WBK])
                        wgg, wggB = wl(wblk, win_v[:, :, C_GG + blk * WBK:C_GG + (blk + 1) * WBK])
                        wpa, wpaB = wl(wblk2, wap_v[:, :, blk * WBK:(blk + 1) * WBK])
                        wpg, wpgB = wl(wblk2, wgp_v[:, :, blk * WBK:(blk + 1) * WBK])
                        for j in range(NJ):
                            dm = blk * NJ + j
                            js = slice(j * 128, (j + 1) * 128)
                            sigs = []
                            for (wg, wgB, col) in ((wga, wgaB, dm), (wgg, wggB, 16 + dm)):
                                pb, pbB_ = nextbank()
                                for dt in range(16):
                                    mm(pb[:], wg[:, dt, js], xsTc[:, dt, :], dt == 0, dt == 15,
                                       reads=[wgB] + xsTcB, writes=[pbB_])
                                sg_, sgB = sig_r.next()
                                act(sg_[:], pb[:], AF.Sigmoid, reads=[pbB_, CB], writes=[sgB], bias=bgT[:, col:col + 1])
                                sigs.append((sg_, sgB))
                            ms = []
                            for k, (wp, wpB) in enumerate(((wpa, wpaB), (wpg, wpgB))):
                                pb, pbB_ = nextbank()
                                for kt in range(8):
                                    if k == 0:
                                        rhs = QA[:, kt, c * TC:(c + 1) * TC]
                                        rds = [wpB] + [QAB[kt // 4][c * NTC + u] for u in range(NTC)]
                                    else:
                                        rhs = sguT[:, kt, :]
                                        rds = [wpB, sguB]
                                    mm(pb[:], wp[:, kt, js], rhs, kt == 0, kt == 7, reads=rds, writes=[pbB_])
                                m1, m1B = m1_r.next()
                                tt(m1[:], sigs[k][0][:], pb[:], ALU.mult, reads=[sigs[k][1], pbB_], writes=[m1B])
                                ms.append((m1, m1B))
                            tt(mT[:, dm, :], ms[0][0][:], ms[1][0][:], ALU.add, reads=[ms[0][1], ms[1][1]],
                               writes=[mTB[dm]])
                    if dbg:
                        final_ops.append(dma(MTd[:, :, c * TC:(c + 1) * TC], mT[:], reads=mTB))
                    for cb in range(D // WBK):
                        wo, woB = wl(wblk, wout_v[:, :, cb * WBK:(cb + 1) * WBK])
                        for tl in range(NTC):
                            row = c * TC + tl * 128
                            xr, xrB = xr_r.next()
                            dma(xr[:], x_own[row:row + 128, cb * WBK:(cb + 1) * WBK], writes=[xrB])
                            pb, pbB_ = nextbank()
                            for dm in range(16):
                                mm(pb[:, 0:WBK], mT[:, dm, tl * 128:(tl + 1) * 128], wo[:, dm, :], dm == 0, dm == 15,
                                   reads=[woB, mTB[dm]], writes=[pbB_])
                            h2t, h2B_ = h2_r.next()
                            tt(h2t[:], xr[:], pb[:, 0:WBK], ALU.add, reads=[xrB, pbB_], writes=[h2B_])
                            o = dma(H2[row:row + 128, cb * WBK:(cb + 1) * WBK], h2t[:], reads=[h2B_],
                                    writes=[H2B[c * NTC + tl]])
                            if dbg:
                                final_ops.append(o)
            P.barrier()

        esQA.close()
        if "E" in phases:
            with ExitStack() as es:
                def sb(name, shape, dt):
                    return es.enter_context(nc.sbuf_tensor("E_" + name, list(shape), dt))

                def ps(name, shape, dt):
                    return es.enter_context(nc.psum_tensor("E_" + name, list(shape), dt))

                NTC = TC // 128
                AG = 64
                NAG = 128 // AG
                KxT = sb("KxT", [128, 16, 128], F32)
                with ExitStack() as es2:
                    ktmp = es2.enter_context(nc.sbuf_tensor("ktmp", [128, 16, 128], F32))
                    pk_ = es2.enter_context(nc.psum_tensor("pk_", [128, 16, 128], F32))
                    b = Buf()
                    kview = ktmp[:].rearrange("p (h two) d -> p h two d", two=2)
                    dma(kview[:, :, 0, :], pk1.rearrange("h n d -> n h d"), writes=[b])
                    dma(kview[:, :, 1, :], pk2.rearrange("h n d -> n h d"), writes=[b])
                    for j in range(16):
                        tr(pk_[:, j, :], ktmp[:, j, :], ident_f[:], reads=[b, CB], writes=[b])
                    cp(KxT[:], pk_[:], reads=[b], writes=[CB])
                    P.barrier()
                R = {
                    "st": Ring(sb, "st", [128, 4], F32, 2),
                    "xs": Ring(sb, "xs", [128, D], BF16, 1),
                    "pT": Ring(ps, "pT", [128, 16, 128], BF16, 2),
                }
                Gt = sb("Gt", [128, AG * TC], BF16)
                G3 = Gt[:].rearrange("p (a t) -> p a t", t=TC)
                qpT = Gt[:].bitcast(F32).rearrange("p (j t) -> p j t", t=TC)
                GB = Buf("G")
                oacc = sb("oacc", [128, NTC, D], F32)
                oaccB = [Buf() for _ in range(NTC)]
                xn2T = sb("xn2T", [128, 16, TC], BF16)
                xn2B = [Buf() for _ in range(NTC)]
                rT = sb("rT", [128, 3, TC], F32)
                rTB = Buf()
                wpq_r = Ring(sb, "wpq", [128, 16, 128], BF16, 2)
                ub_r = Ring(sb, "ub", [128, D], BF16, 2)
                uT_r = Ring(sb, "uT", [128, 16, 128], BF16, 3)
                vb_r = Ring(sb, "vblk", [128, 512], BF16, 6)
                hb_r = Ring(sb, "hb", [128, TC], BF16, 2)
                ohb_r = Ring(sb, "ohb", [128, 8, 128], BF16, 2)
                oha_r = Ring(sb, "oha", [128, 8, AG], BF16, 3)
                vv = sb("vv", [128, 16, 16], F32)
                vi = sb("vi", [128, 16, 16], U32)
                vif = sb("vif", [128, 16, 16], F32)
                wk = sb("wk", [128, 16, 128], F32)
                cand = sb("cand", [128, 8, 256], F32)
                cwk = sb("cwk", [128, 8, 256], F32)
                cv = sb("cv", [128, 8, 16], F32)
                ci = sb("ci", [128, 8, 16], U32)
                cih = sb("cih", [128, 8, 16], U32)
                cil = sb("cil", [128, 8, 16], U32)
                cihf = sb("cihf", [128, 8, 16], F32)
                cilf = sb("cilf", [128, 8, 16], F32)
                abg = sb("abg", [128, 3, 128], F32)
                zz = sb("zz", [128, 16], F32)
                RB = Buf("route")
                L1B = [Buf() for _ in range(16)]
                L2B = [Buf() for _ in range(8)]
                w_pq_v = WPQb.rearrange("(dt p) n -> p dt n", p=128)
                pb4 = [ps("pe%d" % k, [128, 512], F32) for k in range(4)]
                pb4B = [Buf() for _ in range(4)]
                pei = [0]

                def nextbank4(lo=0, hi=4):
                    k = lo + pei[0] % (hi - lo)
                    pei[0] += 1
                    return pb4[k], pb4B[k]

                for c in range(NCH):
                    for tl in range(NTC):
                        row = c * TC + tl * 128
                        dma(oacc[:, tl, :], H2[row:row + 128, :], reads=[H2B[c * NTC + tl]], writes=[oaccB[tl]])

                    for tl in range(NTC):
                        norm_T_sb(oacc[:, tl, :], oaccB[tl], gffnT, xn2T[:, :, tl * 128:(tl + 1) * 128], xn2B[tl], R)
                    for j in range(16):
                        wb, wbB = wpq_r.next()
                        dma(wb[:], w_pq_v[:, :, j * 128:(j + 1) * 128], reads=[WcB], writes=[wbB], q=POOL)
                        pb, pbB_ = nextbank4()
                        for dt in range(16):
                            mm(pb[:], wb[:, dt, :], xn2T[:, dt, :], dt == 0, dt == 15, reads=[wbB] + xn2B, writes=[pbB_])
                        act(qpT[:, j, :], pb[:], AF.Copy, reads=[pbB_], writes=[GB])
                    for tl in range(NTC):
                        tsl = slice(tl * 128, (tl + 1) * 128)
                        sbk = []
                        for q4 in range(4):
                            pb, pbB_ = nextbank4(0, 4)
                            sbk.append((pb, pbB_))
                            for jj in range(4):
                                j = q4 * 4 + jj
                                mm(pb[:, jj * 128:(jj + 1) * 128], qpT[:, j, tsl], KxT[:, j, :], True, True,
                                   reads=[GB, CB], writes=[pbB_])

                        def sc(j):
                            return sbk[j // 4][0][:, (j % 4) * 128:(j % 4 + 1) * 128], sbk[j // 4][1]
                        for j in range(16):
                            s_, sB = sc(j)
                            P.add(DVE, lambda e, j=j, s_=s_: e.max(out=vv[:, j, 0:8], in_=s_), reads=[sB], writes=[L1B[j]])
                        for j in range(16):
                            s_, sB = sc(j)
                            P.add(DVE, lambda e, j=j, s_=s_: e.max_index(out=vi[:, j, 0:8], in_max=vv[:, j, 0:8], in_values=s_),
                                  reads=[sB, L1B[j]], writes=[L1B[j]])
                        for j in range(16):
                            s_, sB = sc(j)
                            P.add(DVE, lambda e, j=j, s_=s_: e.match_replace(out=wk[:, j, :], in_to_replace=vv[:, j, 0:8],
                                                                            in_values=s_, imm_value=-1e30),
                                  reads=[sB, L1B[j]], writes=[L1B[j]])
                        for j in range(16):
                            P.add(DVE, lambda e, j=j: e.max(out=vv[:, j, 8:16], in_=wk[:, j, :]), reads=[L1B[j]], writes=[L1B[j]])
                        for j in range(16):
                            P.add(DVE, lambda e, j=j: e.max_index(out=vi[:, j, 8:16], in_max=vv[:, j, 8:16], in_values=wk[:, j, :]),
                                  reads=[L1B[j]], writes=[L1B[j]])
                        cp(vif[:], vi[:], reads=L1B, writes=[RB])
                        tt(cand[:].rearrange("p h (i j) -> p h i j", j=16),
                           cap(vv[:, 0, :], [(32, 8), (1, 16), (0, 16)]),
                           cap(vv[:, 1, :], [(32, 8), (0, 16), (1, 16)]), ALU.add, reads=L1B, writes=[RB])
                        for h in range(8):
                            P.add(DVE, lambda e, h=h: e.max(out=cv[:, h, 0:8], in_=cand[:, h, :]), reads=[RB], writes=[L2B[h]])
                        for h in range(8):
                            P.add(DVE, lambda e, h=h: e.max_index(out=ci[:, h, 0:8], in_max=cv[:, h, 0:8], in_values=cand[:, h, :]),
                                  reads=[RB, L2B[h]], writes=[L2B[h]])
                        for h in range(8):
                            P.add(DVE, lambda e, h=h: e.match_replace(out=cwk[:, h, :], in_to_replace=cv[:, h, 0:8],
                                                                      in_values=cand[:, h, :], imm_value=-1e30),
                                  reads=[RB, L2B[h]], writes=[L2B[h]])
                        for h in range(8):
                            P.add(DVE, lambda e, h=h: e.max(out=cv[:, h, 8:16], in_=cwk[:, h, :]), reads=[L2B[h]], writes=[L2B[h]])
                        for h in range(8):
                            P.add(DVE, lambda e, h=h: e.max_index(out=ci[:, h, 8:16], in_max=cv[:, h, 8:16], in_values=cwk[:, h, :]),
                                  reads=[L2B[h]], writes=[L2B[h]])
                        P.add(DVE, lambda e: e.tensor_single_scalar(out=cih[:], in_=ci[:], scalar=4, op=ALU.logical_shift_right),
                              reads=L2B, writes=[RB])
                        P.add(DVE, lambda e: e.tensor_single_scalar(out=cil[:], in_=ci[:], scalar=15, op=ALU.bitwise_and),
                              reads=L2B, writes=[RB])
                        cp(cihf[:], cih[:], reads=[RB], writes=[RB])
                        cp(cilf[:], cil[:], reads=[RB], writes=[RB])
                        oh = wk[:].rearrange("p j n -> p (j n)")
                        oh4 = oh.rearrange("p (h k i) -> p h k i", k=16, i=16)
                        for which, (sel, tab) in enumerate(((cihf, 0), (cilf, 1))):
                            tt(oh4, cap(iota_f[:, 0:16], [(0, 8), (0, 16), (1, 16)]),
                               cap(sel[:, 0, :], [(16, 8), (1, 16), (0, 16)]), ALU.is_equal, reads=[RB, CB], writes=[RB] + L1B)
                            tt(oh4, oh4, cap(vif[:, tab, :], [(32, 8), (0, 16), (1, 16)]), ALU.mult, reads=[RB], writes=[RB] + L1B)
                            red(abg[:, which, :], oh.rearrange("p (hk i) -> p hk i", i=16), reads=[RB] + L1B, writes=[RB])
                        tt(cwk[:, :, 0:16], cv[:], cap(cv[:, 0, 0:1], [(16, 8), (0, 16)]), ALU.subtract, reads=L2B, writes=[RB] + L2B)
                        act(cwk[:, :, 0:16], cwk[:, :, 0:16], AF.Exp, reads=[RB], writes=[RB] + L2B)
                        red(zz[:, 0:8], cwk[:, :, 0:16], reads=[RB], writes=[RB])
                        rcp(zz[:, 8:16], zz[:, 0:8], reads=[RB], writes=[RB])
                        tt(abg[:, 2, :].rearrange("p (h k) -> p h k", k=16), cwk[:, :, 0:16],
                           cap(zz[:, 8:16], [(1, 8), (0, 16)]), ALU.mult, reads=[RB], writes=[RB])
                        pTt, pbB_ = R["pT"].next()
                        pb = pTt[:].rearrange("p a b -> p (a b)").bitcast(F32)
                        for w3 in range(3):
                            tr(pb[:, w3 * 128:(w3 + 1) * 128], abg[:, w3, :], ident_f[:], reads=[RB, CB], writes=[pbB_])
                        cp(rT[:, :, tsl], pb[:, 0:384].rearrange("p (w t) -> p w t", t=128), reads=[pbB_], writes=[rTB])
                    if dbg:
                        final_ops.append(dma(RTd[:, :, c * TC:(c + 1) * TC].rearrange("w p t -> p w t"), rT[:], reads=[rTB]))
                    P.barrier()
                    for ag in range(NAG):
                        ioT, ioB = R["pT"].next()
                        ioT2, ioB2 = R["pT"].next()
                        io_b = ioT[:].rearrange("p a b -> p (a b)").bitcast(F32).rearrange("p (t b) -> p t b", b=128)
                        io_a = ioT2[:].rearrange("p a b -> p (a b)").bitcast(F32)[:, 0:8 * AG].rearrange("p (t a) -> p t a", a=AG)
                        cp(io_b, cap(iota_f[:, 0:128], [(0, 8), (1, 128)]), reads=[CB], writes=[ioB])
                        cp(io_a, cap(iota_f[:, ag * AG:(ag + 1) * AG], [(0, 8), (1, AG)]), reads=[CB], writes=[ioB2])
                        for t0 in range(0, TC, 8):
                            ohb, ohbB = ohb_r.next()
                            oha, ohaB = oha_r.next()
                            pb, pbB_ = nextbank4(0, 4)
                            tt(ohb[:], io_b, cap(rT[:, 1, t0:t0 + 8], [(1, 8), (0, 128)]), ALU.is_equal,
                               reads=[rTB, ioB], writes=[ohbB])
                            tt(oha[:], io_a, cap(rT[:, 0, t0:t0 + 8], [(1, 8), (0, AG)]), ALU.is_equal,
                               reads=[rTB, ioB2], writes=[ohaB])
                            tt(oha[:], oha[:], cap(rT[:, 2, t0:t0 + 8], [(1, 8), (0, AG)]), ALU.mult,
                               reads=[rTB], writes=[ohaB], eng=POOL)
                            for u in range(8):
                                mm(pb[:, u * AG:(u + 1) * AG], ohb[:, u, :], oha[:, u, :], True, True,
                                   reads=[ohbB, ohaB], writes=[pbB_])
                            act(G3[:, :, t0:t0 + 8], pb[:, 0:8 * AG].rearrange("p (t a) -> p a t", a=AG), AF.Copy,
                                reads=[pbB_], writes=[GB])
                        def e2_front(al):
                            a = ag * AG + al
                            uT, uTB = uT_r.next()
                            if c > 0:
                                dma(uT[:].rearrange("p a b -> p (a b)"), UT16[a], reads=[UTB[a]], writes=[uTB], q=POOL)
                                return uT, uTB
                            ub, ubB = ub_r.next()
                            dma(ub[:], UB16[a * 128:(a + 1) * 128, :], reads=[UcB[a // 4]], writes=[ubB], q=POOL)
                            pT, pTB = R["pT"].next()
                            for dt in range(16):
                                tr(pT[:, dt, :], ub[:, dt * 128:(dt + 1) * 128], ident_b[:], reads=[ubB, CB], writes=[pTB])
                            cp(uT[:], pT[:], reads=[pTB], writes=[uTB])
                            dma(UT16[a], uT[:].rearrange("p a b -> p (a b)"), reads=[uTB], writes=[UTB[a]])
                            return uT, uTB

                        fr = e2_front(0)
                        for al in range(AG):
                            nfr = e2_front(al + 1) if al + 1 < AG else None
                            uT, uTB = fr
                            pb, pbB_ = nextbank4(0, 2)
                            for dt in range(16):
                                mm(pb[:], uT[:, dt, :], xn2T[:, dt, :], dt == 0, dt == 15, reads=[uTB] + xn2B, writes=[pbB_])
                            hb, hbB = hb_r.next()
                            act(hb[:], pb[:], AF.Gelu_apprx_tanh, reads=[pbB_], writes=[hbB])
                            tt(G3[:, al, :], hb[:], G3[:, al, :], ALU.mult, reads=[hbB, GB], writes=[GB])
                            fr = nfr
                        for db in range(4):
                            banks = [(pb4[tl], pb4B[tl]) for tl in range(NTC)]
                            for al in range(AG):
                                a = ag * AG + al
                                vb, vbB = vb_r.next()
                                dma(vb[:], VB16[a * 128:(a + 1) * 128, db * 512:(db + 1) * 512], reads=[VcB[a // 4]], writes=[vbB])
                                for tl in range(NTC):
                                    mm(banks[tl][0][:], G3[:, al, tl * 128:(tl + 1) * 128], vb[:], al == 0, al == AG - 1,
                                       reads=[GB, vbB], writes=[banks[tl][1]])
                            for tl in range(NTC):
                                osl = oacc[:, tl, db * 512:(db + 1) * 512]
                                tt(osl, osl, banks[tl][0][:], ALU.add, reads=[banks[tl][1]], writes=[oaccB[tl]])
                    for tl in range(NTC):
                        row = c * TC + tl * 128
                        final_ops.append(dma(out[row:row + 128, :], oacc[:, tl, :], reads=[oaccB[tl]]))
            P.barrier()

        if not final_ops:
            final_ops.append(P.all_ops[-1])
        P.prepare(eng_sems, dma_sems, final_ops)
        block = es0.enter_context(nc.Block())
        for nm, en in (("sync", SP), ("tensor", PE), ("scalar", ACT), ("vector", DVE), ("gpsimd", POOL)):
            getattr(block, nm)(lambda e, en=en: P.emit(en, e))
    return nc


def rope_cs(S):
    rows = S // 64
    row = np.repeat(np.arange(rows, dtype=np.float32), 64)
    col = np.tile(np.arange(64, dtype=np.float32), rows)
    inv = (np.float32(10000.0) ** (-np.arange(32, dtype=np.float32) / np.float32(32))).astype(np.float32)
    ang = np.concatenate([row[:, None] * inv, col[:, None] * inv], axis=-1).astype(np.float32)
    return np.concatenate([np.cos(ang), np.sin(ang)], axis=-1).astype(np.float32)


def make_in_maps(inputs, S, NC):
    TOK = S // NC
    f = lambda a: np.ascontiguousarray(np.asarray(a, dtype=np.float32))
    x = f(inputs["x"]).reshape(-1, D)[:S]
    cs = rope_cs(S)
    shared = {
        "x": x, "cs": cs,
        "norm_mix": f(inputs["norm_mix"]).reshape(16, 128),
        "w_in": f(inputs["w_in"]).reshape(D, 7680),
        "b_gate": f(inputs["b_gate"]).reshape(32, 128),
        "q_norm": f(inputs["q_norm"]).reshape(1, 128),
        "k_norm": f(inputs["k_norm"]).reshape(1, 128),
        "sgu_norm": f(inputs["sgu_norm"]).reshape(1, 1024),
        "w_sgu": f(inputs["w_sgu"]).reshape(8, 128, 128),
        "b_sgu": f(inputs["b_sgu"]).reshape(1, 1024),
        "w_attn_proj": f(inputs["w_attn_proj"]).reshape(1024, D),
        "w_sgu_proj": f(inputs["w_sgu_proj"]).reshape(1024, D),
        "w_out": f(inputs["w_out"]).reshape(D, D),
        "norm_ffn": f(inputs["norm_ffn"]).reshape(16, 128),
        "w_peer_q": f(inputs["w_peer_q"]).reshape(D, D),
        "peer_k1": f(inputs["peer_k1"]).reshape(8, 128, 128),
        "peer_k2": f(inputs["peer_k2"]).reshape(8, 128, 128),
        "peer_u": f(inputs["peer_u"]).reshape(NEXP, D),
        "peer_v": f(inputs["peer_v"]).reshape(NEXP, D),
    }
    maps = []
    for c in range(NC):
        m = dict(shared)
        m["x_own"] = np.ascontiguousarray(x[c * TOK:(c + 1) * TOK])
        m["cs_own"] = np.ascontiguousarray(cs[c * TOK:(c + 1) * TOK])
        maps.append(m)
    return maps


_NC_CACHE = {}


def kernel(**inputs):
    S, NC = 16384, 8
    if "nc" not in _NC_CACHE:
        _NC_CACHE["nc"] = build(S, NC)
    nc = _NC_CACHE["nc"]
    in_maps = make_in_maps(inputs, S, NC)
    res = run_bass_kernel_spmd(nc, in_maps, core_ids=list(range(NC)))
    outs = [np.asarray(r["out"], dtype=np.float32) for r in res.results]
    return np.concatenate(outs, axis=0).reshape(1, S, D)
```

```python
import numpy as np
from contextlib import ExitStack
import concourse.bass as bass
import concourse.mybir as mybir
from concourse.bass_utils import run_bass_kernel_spmd

F32 = mybir.dt.float32
BF16 = mybir.dt.bfloat16
U32 = mybir.dt.uint32
AF = mybir.ActivationFunctionType
ALU = mybir.AluOpType
AX = mybir.AxisListType

PE, ACT, DVE, POOL, SP = "tensor", "scalar", "vector", "gpsimd", "sync"
ENGINES = (PE, ACT, DVE, POOL, SP)
N_DMA_SEMS = 48

D = 2048
EPS = 1e-6
C_Q, C_K, C_V, C_SU, C_SV, C_GA, C_GG = 0, 1024, 1280, 1536, 2560, 3584, 5632
NEXP = 16384


class Buf:
    __slots__ = ("name", "last_write", "readers")

    def __init__(self, name=""):
        self.name = name
        self.last_write = None
        self.readers = []


class Op:
    __slots__ = ("eng", "fn", "deps", "is_dma", "signal", "sem", "val")

    def __init__(self, eng, fn, is_dma):
        self.eng = eng
        self.fn = fn
        self.deps = []
        self.is_dma = is_dma
        self.signal = False
        self.sem = None
        self.val = None


class Prog:
    def __init__(self):
        self.ops = {e: [] for e in ENGINES}
        self.all_ops = []
        self.pending = {e: None for e in ENGINES}
        self.dma_since = []

    def add(self, eng, fn, reads=(), writes=(), deps=(), dma=False):
        op = Op(eng, fn, dma)
        ds = []
        for b in reads:
            if b.last_write is not None:
                ds.append(b.last_write)
        for b in writes:
            if b.last_write is not None:
                ds.append(b.last_write)
            ds.extend(b.readers)
        ds.extend(d for d in deps if d is not None)
        if self.pending[eng] is not None:
            ds.extend(self.pending[eng])
            self.pending[eng] = None
        seen = set()
        for d in ds:
            if id(d) in seen:
                continue
            seen.add(id(d))
            if (not d.is_dma) and (not dma) and d.eng == eng and eng == PE:
                continue
            op.deps.append(d)
            d.signal = True
        for b in reads:
            b.readers.append(op)
        for b in writes:
            b.last_write = op
            b.readers = []
        self.ops[eng].append(op)
        self.all_ops.append(op)
        if dma:
            self.dma_since.append(op)
        return op

    def barrier(self):
        last = []
        for e in ENGINES:
            for o in reversed(self.ops[e]):
                if not o.is_dma:
                    last.append(o)
                    break
        last.extend(self.dma_since)
        self.dma_since = []
        for e in ENGINES:
            self.pending[e] = list(last)

    def prepare(self, eng_sems, dma_sems, final_ops):
        for o in final_ops:
            o.signal = True
        cnt = {e: 0 for e in ENGINES}
        dma_tot = [0] * len(dma_sems)
        dma_last = [None] * len(dma_sems)
        n_all = len(dma_sems)
        pools = {SP: (0, n_all // 2), ACT: (n_all // 2, n_all // 2 + n_all // 8),
                 POOL: (n_all // 2 + n_all // 8, n_all)}
        rrq = {SP: 0, ACT: 0, POOL: 0}
        for op in self.all_ops:
            if op.is_dma:
                lo, hi = pools[op.eng]
                k = lo + rrq[op.eng] % (hi - lo)
                rrq[op.eng] += 1
                if dma_last[k] is not None:
                    op.deps.append(dma_last[k])
                dma_tot[k] += 16
                op.sem = dma_sems[k]
                op.val = dma_tot[k]
                dma_last[k] = op
            elif op.signal:
                cnt[op.eng] += 1
                op.sem = eng_sems[op.eng]
                op.val = cnt[op.eng]
        self.final_ops = final_ops

    def emit(self, eng, e):
        waited = {}
        for op in self.ops[eng]:
            need = {}
            for d in op.deps:
                key = id(d.sem)
                if key not in need or need[key][1] < d.val:
                    need[key] = (d.sem, d.val)
            for key, (sem, val) in need.items():
                if waited.get(key, 0) >= val:
                    continue
                e.wait_ge(sem, val)
                waited[key] = val
            ins = op.fn(e)
            if op.is_dma:
                ins.then_inc(op.sem, 16)
            elif op.signal:
                ins.then_inc(op.sem, 1)
        if eng == SP:
            for o in self.final_ops:
                key = id(o.sem)
                if waited.get(key, 0) < o.val:
                    e.wait_ge(o.sem, o.val)
                    waited[key] = o.val


class Ring:
    def __init__(self, alloc, name, shape, dt, n):
        self.tiles = [alloc("%s%d" % (name, i), shape, dt) for i in range(n)]
        self.bufs = [Buf("%s%d" % (name, i)) for i in range(n)]
        self.i = 0

    def next(self):
        k = self.i % len(self.tiles)
        self.i += 1
        return self.tiles[k], self.bufs[k]


def cap(ap, dims):
    return bass.AP(ap.tensor, ap.offset, [list(ap.ap[0])] + [[s, n] for s, n in dims])


def build(S, NC, dbg=False, phases="ABCE"):
    NT_ALL = S // 128
    TOK = S // NC
    NT_OWN = TOK // 128
    TC = 512
    NCH = TOK // TC
    nc = bass.Bass("TRN2", target_bir_lowering=False)
    P = Prog()

    def din(name, shape, dt=F32):
        return nc.dram_tensor(name, list(shape), dt, kind="ExternalInput").ap()

    x_all = din("x", [S, D])
    x_own = din("x_own", [TOK, D])
    cs_all = din("cs", [S, 128])
    cs_own = din("cs_own", [TOK, 128])
    norm_mix = din("norm_mix", [16, 128])
    w_in = din("w_in", [D, 7680])
    b_gate = din("b_gate", [32, 128])
    q_norm = din("q_norm", [1, 128])
    k_norm = din("k_norm", [1, 128])
    sgu_norm = din("sgu_norm", [1, 1024])
    w_sgu = din("w_sgu", [8, 128, 128])
    b_sgu = din("b_sgu", [1, 1024])
    w_ap = din("w_attn_proj", [1024, D])
    w_gp = din("w_sgu_proj", [1024, D])
    w_out = din("w_out", [D, D])
    norm_ffn = din("norm_ffn", [16, 128])
    w_pq = din("w_peer_q", [D, D])
    pk1 = din("peer_k1", [8, 128, 128])
    pk2 = din("peer_k2", [8, 128, 128])
    peer_u = din("peer_u", [NEXP, D])
    peer_v = din("peer_v", [NEXP, D])
    out = nc.dram_tensor("out", [TOK, D], F32, kind="ExternalOutput").ap()
    skind = "ExternalOutput" if dbg else "Internal"
    KT = nc.dram_tensor("KT", [2, 128, S], BF16, kind=skind).ap()
    VD = nc.dram_tensor("VD", [2, S, 128], BF16, kind=skind).ap()
    H2 = nc.dram_tensor("H2", [TOK, D], F32, kind=skind).ap()
    WINb = nc.dram_tensor("WINb", [D, 7680], BF16, kind="Internal").ap()
    WAPb = nc.dram_tensor("WAPb", [1024, D], BF16, kind="Internal").ap()
    WGPb = nc.dram_tensor("WGPb", [1024, D], BF16, kind="Internal").ap()
    WOUTb = nc.dram_tensor("WOUTb", [D, D], BF16, kind="Internal").ap()
    WPQb = nc.dram_tensor("WPQb", [D, D], BF16, kind="Internal").ap()
    UB16 = nc.dram_tensor("UB16", [NEXP, D], BF16, kind="Internal").ap()
    VB16 = nc.dram_tensor("VB16", [NEXP, D], BF16, kind="Internal").ap()
    WcB = Buf("wcast")
    UcB = [Buf() for _ in range(NEXP // 512)]
    VcB = [Buf() for _ in range(NEXP // 512)]
    if dbg:
        QAd = nc.dram_tensor("QAd", [128, 8, TOK], BF16, kind="ExternalOutput").ap()
        ATd = nc.dram_tensor("ATd", [128, 8, TOK], BF16, kind="ExternalOutput").ap()
        SGd = nc.dram_tensor("SGd", [128, 8, TOK], BF16, kind="ExternalOutput").ap()
        MTd = nc.dram_tensor("MTd", [128, 16, TOK], BF16, kind="ExternalOutput").ap()
        RTd = nc.dram_tensor("RTd", [3, 128, TOK], F32, kind="ExternalOutput").ap()
    KTB = [[Buf() for _ in range(NT_ALL)] for _ in range(2)]
    VDB = [[Buf() for _ in range(NT_ALL)] for _ in range(2)]
    H2B = [Buf() for _ in range(NT_OWN)]
    final_ops = []

    with ExitStack() as es0:
        def sb0(name, shape, dt):
            return es0.enter_context(nc.sbuf_tensor(name, list(shape), dt))

        eng_sems = {e: es0.enter_context(nc.semaphore("s_" + e)) for e in ENGINES}
        dma_sems = [es0.enter_context(nc.semaphore("d%d" % i)) for i in range(N_DMA_SEMS)]

        def dma(o, i, reads=(), writes=(), q=SP):
            return P.add(q, lambda e: e.dma_start(out=o, in_=i), reads, writes, dma=True)

        def mm(o, lhsT, rhs, start, stop, reads=(), writes=()):
            return P.add(PE, lambda e: e.matmul(o, lhsT=lhsT, rhs=rhs, start=start, stop=stop), reads, writes)

        def tr(o, i, ident, reads=(), writes=()):
            return P.add(PE, lambda e: e.transpose(out=o, in_=i, identity=ident), reads, writes)

        def act(o, i, func, reads=(), writes=(), **kw):
            return P.add(ACT, lambda e: e.activation(out=o, in_=i, func=func, **kw), reads, writes)

        def tt(o, a, b, op, reads=(), writes=(), eng=DVE):
            return P.add(eng, lambda e: e.tensor_tensor(out=o, in0=a, in1=b, op=op), reads, writes)

        def ts(o, a, s1, s2, op0, op1=None, reads=(), writes=(), eng=DVE):
            if op1 is None:
                return P.add(eng, lambda e: e.tensor_scalar(out=o, in0=a, scalar1=s1, scalar2=None, op0=op0), reads, writes)
            return P.add(eng, lambda e: e.tensor_scalar(out=o, in0=a, scalar1=s1, scalar2=s2, op0=op0, op1=op1), reads, writes)

        def cp(o, i, reads=(), writes=(), eng=DVE):
            return P.add(eng, lambda e: e.tensor_copy(out=o, in_=i), reads, writes)

        def red(o, i, reads=(), writes=()):
            return P.add(DVE, lambda e: e.tensor_reduce(out=o, in_=i, axis=AX.X, op=ALU.add), reads, writes)

        def rcp(o, i, reads=(), writes=()):
            return P.add(DVE, lambda e: e.reciprocal(out=o, in_=i), reads, writes)

        ident_f = sb0("ident_f", [128, 128], F32)
        ident_b = sb0("ident_b", [128, 128], BF16)
        iota_b = sb0("iota_b", [128, 128], BF16)
        iota_f = sb0("iota_f", [128, 128], F32)
        iota_p = sb0("iota_p", [128, 1], F32)
        gmixT = sb0("gmixT", [128, 16], F32)
        gffnT = sb0("gffnT", [128, 16], F32)
        CB = Buf("consts")
        P.add(POOL, lambda e: e.iota(iota_p[:], pattern=[[0, 1]], base=0, channel_multiplier=1,
                                     allow_small_or_imprecise_dtypes=True), writes=[CB])
        P.add(POOL, lambda e: e.iota(iota_f[:], pattern=[[1, 128]], base=0, channel_multiplier=0,
                                     allow_small_or_imprecise_dtypes=True), writes=[CB])
        ts(ident_f[:], iota_f[:], iota_p[:, 0:1], None, ALU.is_equal, reads=[CB], writes=[CB])
        cp(ident_b[:], ident_f[:], reads=[CB], writes=[CB])
        cp(iota_b[:], iota_f[:], reads=[CB], writes=[CB])

        esQA = ExitStack()
        QA = esQA.enter_context(nc.sbuf_tensor("QA", [128, 8, TOK], BF16))
        QAB = [[Buf() for _ in range(NT_OWN)] for _ in range(2)]

        def load_colvec(dst, src, rows, es):
            tmp = es.enter_context(nc.sbuf_tensor("cv_tmp_%s" % dst.name, [rows, 128], F32))
            ptmp = es.enter_context(nc.psum_tensor("cv_ps_%s" % dst.name, [128, rows], F32))
            b = Buf()
            dma(tmp[:], src, writes=[b])
            tr(ptmp[:], tmp[:], ident_f[0:rows, 0:rows], reads=[b, CB], writes=[b])
            cp(dst[:], ptmp[:], reads=[b], writes=[CB])

        with ExitStack() as es:
            load_colvec(gmixT, norm_mix, 16, es)
            load_colvec(gffnT, norm_ffn, 16, es)
        P.barrier()

        def norm_T(src, gT, dst, dstB, R):
            xt, xtB = R["xt"].next()
            dma(xt[:], src, writes=[xtB])
            return norm_T_sb(xt[:], xtB, gT, dst, dstB, R)

        def norm_T_sb(xt, xtB, gT, dst, dstB, R, defer=False, dstB2=None):
            st, stB = R["st"].next()
            xs, xsB = R["xs"].next()
            pT, pTB = R["pT"].next()
            act(xs[:], xt, AF.Square, reads=[xtB], writes=[xsB, stB], accum_out=st[:, 0:1])
            act(st[:, 1:2], st[:, 0:1], AF.Sqrt, reads=[stB], writes=[stB], scale=1.0 / D, bias=EPS)
            rcp(st[:, 2:3], st[:, 1:2], reads=[stB], writes=[stB])
            if defer:
                act(xs[:], xt, AF.Copy, reads=[xtB], writes=[xsB])
            else:
                act(xs[:], xt, AF.Copy, reads=[xtB, stB], writes=[xsB], scale=st[:, 2:3])
            for dt in range(16):
                tr(pT[:, dt, :], xs[:, dt * 128:(dt + 1) * 128], ident_b[:], reads=[xsB, CB], writes=[pTB])
            if dstB2 is None:
                tt(dst, pT[:], cap(gT[:], [(1, 16), (0, 128)]), ALU.mult, reads=[pTB, CB], writes=[dstB])
            else:
                tt(dst[:, 0:8, :], pT[:, 0:8, :], cap(gT[:, 0:8], [(1, 8), (0, 128)]), ALU.mult,
                   reads=[pTB, CB], writes=[dstB])
                tt(dst[:, 8:16, :], pT[:, 8:16, :], cap(gT[:, 8:16], [(1, 8), (0, 128)]), ALU.mult,
                   reads=[pTB, CB], writes=[dstB2])
            return st, stB

        def qk_norm_rope(src, srcB, nh, gain, cst, cstB, kr, krB, R):
            W = nh * 128
            sq, kn, t1, t2, sm = R["sq"], R["kn"], R["t1"], R["t2"], R["sm"]
            TB = R["ropeB"]
            src3 = src.rearrange("p (h d) -> p h d", d=128)
            act(sq[:, 0:W], src, AF.Square, reads=[srcB], writes=[TB])
            red(sm[:, 0:nh], sq[:, 0:W].rearrange("p (h d) -> p h d", d=128), reads=[TB], writes=[TB])
            act(sm[:, 16:16 + nh], sm[:, 0:nh], AF.Sqrt, reads=[TB], writes=[TB], scale=1.0 / 128, bias=EPS)
            rcp(sm[:, 32:32 + nh], sm[:, 16:16 + nh], reads=[TB], writes=[TB])
            kn3 = kn[:, 0:W].rearrange("p (h d) -> p h d", d=128)
            tt(kn3, src3, cap(sm[:, 32:32 + nh], [(1, nh), (0, 128)]), ALU.mult, reads=[srcB, TB], writes=[TB])
            tt(kn3, kn3, gain, ALU.mult, reads=[TB, CB], writes=[TB])
            x1 = kn[:, 0:W:2].rearrange("p (h j) -> p h j", j=64)
            x2 = kn[:, 1:W:2].rearrange("p (h j) -> p h j", j=64)
            c = cap(cst[:, 0:64], [(0, nh), (1, 64)])
            s = cap(cst[:, 64:128], [(0, nh), (1, 64)])
            a1 = t1[:, 0:nh * 64].rearrange("p (h j) -> p h j", j=64)
            a2 = t2[:, 0:nh * 64].rearrange("p (h j) -> p h j", j=64)
            tt(a1, x1, c, ALU.mult, reads=[TB, cstB], writes=[TB])
            tt(a2, x2, s, ALU.mult, reads=[TB, cstB], writes=[TB])
            tt(kr[:, :, 0:128:2], a1, a2, ALU.subtract, reads=[TB], writes=[krB])
            TB2 = R["ropeB2"]
            b1 = R["t3"][:, 0:nh * 64].rearrange("p (h j) -> p h j", j=64)
            b2 = R["t4"][:, 0:nh * 64].rearrange("p (h j) -> p h j", j=64)
            tt(b1, x1, s, ALU.mult, reads=[TB, cstB], writes=[TB2])
            tt(b2, x2, c, ALU.mult, reads=[TB, cstB], writes=[TB2])
            tt(kr[:, :, 1:128:2], b1, b2, ALU.add, reads=[TB2], writes=[krB])

        def wload(dst, src, writes, nsplit=4):
            k = dst.shape[1]
            step = max(1, k // nsplit)
            ops = []
            for a in range(0, k, step):
                ops.append(dma(dst[:, a:a + step, :], src[:, a:a + step, :], writes=writes, q=POOL))
            return ops

        def emit_precasts(deps):
            def precast(dst, src, rows, cols, writes):
                k = 1920 if cols == 7680 else cols
                sv = src.rearrange("r (c k) -> (r c) k", k=k)
                dv = dst.rearrange("r (c k) -> (r c) k", k=k)
                n = rows * (cols // k)
                for a in range(0, n, 512):
                    P.add(POOL, lambda e, o=dv[a:a + 512, :], i=sv[a:a + 512, :]: e.dma_start(out=o, in_=i),
                          (), writes(a), deps=deps, dma=True)
            if "C" in phases:
                precast(WINb, w_in, D, 7680, lambda a: [WcB])
                precast(WAPb, w_ap, 1024, D, lambda a: [WcB])
                precast(WGPb, w_gp, 1024, D, lambda a: [WcB])
                precast(WOUTb, w_out, D, D, lambda a: [WcB])
            if "E" in phases:
                precast(WPQb, w_pq, D, D, lambda a: [WcB])
                precast(UB16, peer_u, NEXP, D, lambda a: [UcB[a // 512]])
                precast(VB16, peer_v, NEXP, D, lambda a: [VcB[a // 512]])

        w_in_v = w_in.rearrange("(dt p) n -> p dt n", p=128)

        if "A" in phases:
            with ExitStack() as es:
                def sb(name, shape, dt):
                    return es.enter_context(nc.sbuf_tensor("A_" + name, list(shape), dt))

                def ps(name, shape, dt):
                    return es.enter_context(nc.psum_tensor("A_" + name, list(shape), dt))

                R = {
                    "xt": Ring(sb, "xt", [128, D], F32, 3),
                    "st": Ring(sb, "st", [128, 4], F32, 4),
                    "xs": Ring(sb, "xs", [128, D], BF16, 2),
                    "pT": Ring(ps, "pT", [128, 16, 128], BF16, 1),
                    "sq": sb("sq", [128, 1024], F32), "kn": sb("kn", [128, 1024], F32),
                    "t1": sb("t1", [128, 512], F32), "t2": sb("t2", [128, 512], F32),
                    "t3": sb("t3", [128, 512], F32), "t4": sb("t4", [128, 512], F32), "ropeB2": Buf(),
                    "sm": sb("sm", [128, 48], F32), "ropeB": Buf(),
                }
                xsT_r = Ring(sb, "xsT", [128, 16, 128], BF16, 2)
                xsT_hiB = [Buf(), Buf()]
                cs_r = Ring(sb, "cst", [128, 128], F32, 4)
                wkv = sb("wkv", [128, 16, 512], BF16)
                wq = sb("wq", [128, 16, 1024], BF16)
                gqk = sb("gqk", [128, 256], F32)
                WB = Buf("wA")
                wload(wkv[:], w_in_v[:, :, C_K:C_K + 512], [WB])
                wload(wq[:], w_in_v[:, :, C_Q:C_Q + 1024], [WB], nsplit=8)
                if "B" not in phases:
                    emit_precasts([])
                dma(gqk[:, 0:128], q_norm[0, :].partition_broadcast(128), writes=[CB])
                dma(gqk[:, 128:256], k_norm[0, :].partition_broadcast(128), writes=[CB])
                pbig = Ring(ps, "pbig", [128, 1024], F32, 2)
                pKT = Ring(ps, "pKT", [128, 8, 128], BF16, 2)
                kr_r = Ring(sb, "kr", [128, 8, 128], BF16, 2)
                kvs_r = Ring(sb, "kvs", [128, 1024], F32, 2)
                vb_r = Ring(sb, "vb", [128, 2, 128], BF16, 3)
                kT_r = Ring(sb, "kTs", [128, 2, 128], BF16, 3)

                def a_stage0(xsrc, cssrc, i):
                    cst, cstB = cs_r.next()
                    dma(cst[:], cssrc[i * 128:(i + 1) * 128, :], writes=[cstB])
                    xt, xtB = R["xt"].next()
                    dma(xt[:], xsrc[i * 128:(i + 1) * 128, :], writes=[xtB])
                    return (cst, cstB, xt, xtB)

                def a1_stage1(i, ld):
                    hiB = xsT_hiB[xsT_r.i % 2]
                    xsT, xsTB = xsT_r.next()
                    cst, cstB, xt, xtB = ld
                    st, stB = norm_T_sb(xt[:], xtB, gmixT, xsT[:], xsTB, R, defer=True, dstB2=hiB)
                    pk, pkB = pbig.next()
                    for dt in range(16):
                        mm(pk[:, 0:512], xsT[:, dt, :], wkv[:, dt, :], dt == 0, dt == 15,
                           reads=[xsTB if dt < 8 else hiB, WB], writes=[pkB])
                    return (i, pk, pkB, cst, cstB, st, stB)

                def a1_stage2(stt):
                    i, pk, pkB, cst, cstB, st, stB = stt
                    kvs, kvsB = kvs_r.next()
                    ts(kvs[:, 0:512], pk[:, 0:512], st[:, 2:3], None, ALU.mult, reads=[pkB, stB], writes=[kvsB])
                    kr, krB = kr_r.next()
                    qk_norm_rope(kvs[:, 0:256], kvsB, 2, cap(gqk[:, 128:256], [(0, 2), (1, 128)]), cst, cstB,
                                 kr[:, 0:2, :], krB, R)
                    vb, vbB = vb_r.next()
                    cp(vb[:].rearrange("p h d -> p (h d)"), kvs[:, 256:512], reads=[kvsB], writes=[vbB], eng=POOL)
                    pt_, ptB = pKT.next()
                    for h in range(2):
                        tr(pt_[:, h, :], kr[:, h, :], ident_b[:], reads=[krB, CB], writes=[ptB])
                    def late():
                        kTs, kTB = kT_r.next()
                        cp(kTs[:], pt_[:, 0:2, :], reads=[ptB], writes=[kTB])
                        dma(KT[:, :, i * 128:(i + 1) * 128].rearrange("h d t -> d h t"), kTs[:], reads=[kTB],
                            writes=[KTB[0][i], KTB[1][i]])
                        dma(VD[:, i * 128:(i + 1) * 128, :].rearrange("h t d -> t h d"), vb[:], reads=[vbB],
                            writes=[VDB[0][i], VDB[1][i]])
                    return late

                def a2_stage1(i, ld):
                    hiB = xsT_hiB[xsT_r.i % 2]
                    xsT, xsTB = xsT_r.next()
                    cst, cstB, xt, xtB = ld
                    st, stB = norm_T_sb(xt[:], xtB, gmixT, xsT[:], xsTB, R, defer=True, dstB2=hiB)
                    pq, pqB = pbig.next()
                    for half in range(2):
                        for dt in range(16):
                            mm(pq[:, half * 512:(half + 1) * 512], xsT[:, dt, :], wq[:, dt, half * 512:(half + 1) * 512],
                               dt == 0, dt == 15, reads=[xsTB if dt < 8 else hiB, WB], writes=[pqB])
                    return (i, pq, pqB, cst, cstB, st, stB)

                def a2_stage2(stt):
                    i, pq, pqB, cst, cstB, st, stB = stt
                    kvs, kvsB = kvs_r.next()
                    ts(kvs[:], pq[:], st[:, 2:3], None, ALU.mult, reads=[pqB, stB], writes=[kvsB])
                    kr, krB = kr_r.next()
                    qk_norm_rope(kvs[:], kvsB, 8, cap(gqk[:, 0:128], [(0, 8), (1, 128)]), cst, cstB, kr[:], krB, R)
                    pt_, ptB = pKT.next()
                    for h in range(8):
                        tr(pt_[:, h, :], kr[:, h, :], ident_b[:], reads=[krB, CB], writes=[ptB])
                    def late():
                        cp(QA[:, :, i * 128:(i + 1) * 128], pt_[:], reads=[ptB], writes=[QAB[0][i], QAB[1][i]])
                    return late

                work = [(a1_stage1, a1_stage2, i, x_all, cs_all) for i in range(NT_ALL)] + \
                       [(a2_stage1, a2_stage2, i, x_own, cs_own) for i in range(NT_OWN)]
                NW = len(work)
                lds = {}
                for w in range(min(2, NW)):
                    lds[w] = a_stage0(work[w][3], work[w][4], work[w][2])
                cur = work[0][0](work[0][2], lds.pop(0))
                late_prev = None
                for w in range(NW):
                    if w + 2 < NW:
                        lds[w + 2] = a_stage0(work[w + 2][3], work[w + 2][4], work[w + 2][2])
                    nxt = work[w + 1][0](work[w + 1][2], lds.pop(w + 1)) if w + 1 < NW else None
                    if late_prev is not None:
                        late_prev()
                    late_prev = work[w][1](cur)
                    cur = nxt
                late_prev()
                if dbg:
                    final_ops.append(dma(QAd, QA[:], reads=[b for r in QAB for b in r]))
            P.barrier()

        if "B" in phases:
            with ExitStack() as es:
                def sb(name, shape, dt):
                    return es.enter_context(nc.sbuf_tensor("B_" + name, list(shape), dt))

                def ps(name, shape, dt):
                    return es.enter_context(nc.psum_tensor("B_" + name, list(shape), dt))

                KTs2 = [sb("KTs%d" % h, [128, S], BF16) for h in range(2)]
                V12 = [sb("V1%d" % h, [128, NT_ALL, 130], BF16) for h in range(2)]
                NSEG = 8 if NT_ALL >= 8 else (4 if NT_ALL >= 4 else 1)
                SEG = NT_ALL // NSEG
                KB2 = [[Buf() for _ in range(NSEG)] for h in range(2)]
                VB2 = [[Buf() for _ in range(NSEG)] for h in range(2)]
                pS = Ring(ps, "pS", [128, 512], F32, 3)
                pO = [ps("pO%d" % g, [128, 512], F32) for g in range(4)]
                pOB = [Buf() for _ in range(4)]
                pOT = Ring(ps, "pOT", [128, 4, 128], BF16, 1)
                pt_r = Ring(sb, "pt", [128, 512], BF16, 4)
                ob_r = Ring(sb, "ob", [128, 128], BF16, 4)
                rc_r = Ring(sb, "rc", [128, 1], F32, 4)
                P.add(POOL, lambda e: e.memset(V12[0][:], 1.0), writes=VB2[0])
                P.add(POOL, lambda e: e.memset(V12[1][:], 1.0), writes=VB2[1])
                kv_loads = []
                for kvh in range(2):
                    for sg in range(NSEG):
                        a, b = sg * SEG, (sg + 1) * SEG
                        kv_loads.append(dma(KTs2[kvh][:, a * 128:b * 128], KT[kvh, :, a * 128:b * 128],
                                            reads=KTB[kvh][a:b], writes=[KB2[kvh][sg]]))
                        kv_loads.append(dma(V12[kvh][:, a:b, 0:128],
                                            VD[kvh, a * 128:b * 128, :].rearrange("(n p) d -> p n d", p=128),
                                            reads=VDB[kvh][a:b], writes=[VB2[kvh][sg]]))
                emit_precasts(kv_loads)
                scale = 1.0 / float(np.sqrt(128.0))
                for kvh in range(2):
                    KTs, V1, KB, VB = KTs2[kvh], V12[kvh], KB2[kvh], VB2[kvh]
                    steps = [(qt, st) for qt in range(NT_OWN) for st in range(NT_ALL)]
                    LA = 2
                    pend = []

                    def emit_qk(idx):
                        qt, st = steps[idx]
                        qsl_ = QA[:, 4 * kvh:4 * kvh + 4, qt * 128:(qt + 1) * 128]
                        p_s, p_sB = pS.next()
                        mm(p_s[:], KTs[:, st * 128:(st + 1) * 128], qsl_, True, True,
                           reads=[KB[st // SEG], QAB[kvh][qt]], writes=[p_sB])
                        pend.append((p_s, p_sB))

                    for idx in range(min(LA, len(steps))):
                        emit_qk(idx)
                    for idx, (qt, st) in enumerate(steps):
                        if idx + LA < len(steps):
                            emit_qk(idx + LA)
                        qB = QAB[kvh][qt]
                        qsl = QA[:, 4 * kvh:4 * kvh + 4, qt * 128:(qt + 1) * 128]
                        sg = st // SEG
                        p_s, p_sB = pend.pop(0)
                        pt, ptB = pt_r.next()
                        act(pt[:], p_s[:], AF.Exp, reads=[p_sB], writes=[ptB], scale=scale)
                        for g in range(4):
                            mm(pO[g][:, 0:129], pt[:, g * 128:(g + 1) * 128], V1[:, st, 0:129], st == 0,
                               st == NT_ALL - 1, reads=[ptB, VB[sg]], writes=[pOB[g]])
                        if st == NT_ALL - 1:
                            po_t, po_tB = pOT.next()
                            for g in range(4):
                                rc, rcB = rc_r.next()
                                ob, obB = ob_r.next()
                                rcp(rc[:], pO[g][:, 128:129], reads=[pOB[g]], writes=[rcB])
                                ts(ob[:], pO[g][:, 0:128], rc[:, 0:1], None, ALU.mult, reads=[pOB[g], rcB], writes=[obB])
                                tr(po_t[:, g, :], ob[:], ident_b[:], reads=[obB, CB], writes=[po_tB])
                            act(qsl, po_t[:], AF.Copy, reads=[po_tB], writes=[qB])
                if dbg:
                    final_ops.append(dma(ATd, QA[:], reads=[b for r in QAB for b in r]))
            P.barrier()

        if "C" in phases:
            with ExitStack() as es:
                def sb(name, shape, dt):
                    return es.enter_context(nc.sbuf_tensor("C_" + name, list(shape), dt))

                def ps(name, shape, dt):
                    return es.enter_context(nc.psum_tensor("C_" + name, list(shape), dt))

                R = {
                    "xt": Ring(sb, "xt", [128, D], F32, 2),
                    "st": Ring(sb, "st", [128, 4], F32, 2),
                    "xs": Ring(sb, "xs", [128, D], BF16, 2),
                    "pT": Ring(ps, "pT", [128, 16, 128], BF16, 1),
                    "junk": sb("junk", [128, 1024], BF16), "junkB": Buf(),
                }
                wsT = sb("wsT", [128, 8, 128], BF16)
                bs_bc = sb("bs_bc", [128, 8, 128], F32)
                gsg = sb("gsg", [128, 1024], F32)
                bgT = sb("bgT", [128, 32], F32)
                with ExitStack() as es2:
                    load_colvec(bgT, b_gate, 32, es2)
                    wtmp = es2.enter_context(nc.sbuf_tensor("wtmp", [128, 8, 128], F32))
                    pw = es2.enter_context(nc.psum_tensor("pw", [128, 8, 128], F32))
                    b = Buf()
                    dma(wtmp[:], w_sgu.rearrange("g p q -> p g q"), writes=[b])
                    for g in range(8):
                        tr(pw[:, g, :], wtmp[:, g, :], ident_f[:], reads=[b, CB], writes=[b])
                    cp(wsT[:], pw[:], reads=[b], writes=[CB])
                    dma(bs_bc[:].rearrange("p g q -> p (g q)"), b_sgu[0, :].partition_broadcast(128), writes=[CB])
                    dma(gsg[:], sgu_norm[0, :].partition_broadcast(128), writes=[CB])
                    P.barrier()
                xsTc = sb("xsTc", [128, 16, TC], BF16)
                xsTcB = [Buf() for _ in range(TC // 128)]
                sguT = sb("sguT", [128, 8, TC], BF16)
                sguB = Buf()
                mT = sb("mT", [128, 16, TC], BF16)
                mTB = [Buf() for _ in range(16)]
                WBK = 256
                NJ = WBK // 128
                wblk = Ring(sb, "wblk", [128, 16, WBK], BF16, 4)
                wblk2 = Ring(sb, "wblk2", [128, 8, WBK], BF16, 4)
                gv = sb("gv", [128, 1024], F32)
                gvB = Buf()
                vn_r = Ring(sb, "vn", [128, 1024], BF16, 2)
                tmpm = sb("tmpm", [128, 8, 128], F32)
                tmpmB = Buf()
                sig_r = Ring(sb, "sig", [128, 512], F32, 4)
                m1_r = Ring(sb, "m1", [128, 512], F32, 4)
                xr_r = Ring(sb, "xr", [128, WBK], F32, 4)
                h2_r = Ring(sb, "h2t", [128, WBK], F32, 3)
                pbk = [ps("pbk%d" % k, [128, 512], F32) for k in range(6)]
                pbB = [Buf() for _ in range(6)]
                pbi = [0]

                def nextbank():
                    k = pbi[0] % 6
                    pbi[0] += 1
                    return pbk[k], pbB[k]

                win_v = WINb.rearrange("(dt p) n -> p dt n", p=128)
                wap_v = WAPb.rearrange("(kt p) n -> p kt n", p=128)
                wgp_v = WGPb.rearrange("(kt p) n -> p kt n", p=128)
                wout_v = WOUTb.rearrange("(kt p) n -> p kt n", p=128)

                def wl(ring, src):
                    wb, wbB = ring.next()
                    dma(wb[:], src, reads=[WcB], writes=[wbB], q=POOL)
                    return wb, wbB

                NTC = TC // 128
                for c in range(NCH):
                    for tl in range(NTC):
                        row = c * TC + tl * 128
                        norm_T(x_own[row:row + 128, :], gmixT, xsTc[:, :, tl * 128:(tl + 1) * 128], xsTcB[tl], R)
                    for blk in range(1024 // WBK):
                        wb, wbB = wl(wblk, win_v[:, :, C_SU + blk * WBK:C_SU + (blk + 1) * WBK])
                        for j in range(NJ):
                            f = blk * NJ + j
                            pb, pbB_ = nextbank()
                            for dt in range(16):
                                mm(pb[:], wb[:, dt, j * 128:(j + 1) * 128], xsTc[:, dt, :], dt == 0, dt == 15,
                                   reads=[wbB] + xsTcB, writes=[pbB_])
                            act(sguT[:, f, :], pb[:], AF.Gelu_apprx_tanh, reads=[pbB_], writes=[sguB])
                    wsv = [wl(wblk, win_v[:, :, C_SV + blk * WBK:C_SV + (blk + 1) * WBK]) for blk in range(1024 // WBK)]
                    for tl in range(NTC):
                        banks = [nextbank(), nextbank()]
                        for blk in range(1024 // WBK):
                            wb, wbB = wsv[blk]
                            pb, pbB_ = banks[(blk * WBK) // 512]
                            co = (blk * WBK) % 512
                            for dt in range(16):
                                mm(pb[:, co:co + WBK], xsTc[:, dt, tl * 128:(tl + 1) * 128], wb[:, dt, :], dt == 0, dt == 15,
                                   reads=[wbB, xsTcB[tl]], writes=[pbB_])
                        st, stB = R["st"].next()
                        for blk in range(2):
                            act(gv[:, blk * 512:(blk + 1) * 512], banks[blk][0][:], AF.Gelu_apprx_tanh,
                                reads=[banks[blk][1]], writes=[gvB])
                        act(R["junk"][:], gv[:], AF.Square, reads=[gvB], writes=[R["junkB"], stB],
                            accum_out=st[:, 0:1])
                        act(st[:, 1:2], st[:, 0:1], AF.Sqrt, reads=[stB], writes=[stB], scale=1.0 / 1024, bias=EPS)
                        rcp(st[:, 2:3], st[:, 1:2], reads=[stB], writes=[stB])
                        vn, vnB = vn_r.next()
                        P.add(DVE, lambda e, vn=vn, st=st: e.scalar_tensor_tensor(
                            out=vn[:], in0=gv[:], scalar=st[:, 2:3], in1=gsg[:], op0=ALU.mult, op1=ALU.mult),
                            reads=[gvB, stB, CB], writes=[vnB])
                        pm0, pm0B = nextbank()
                        pm1, pm1B = nextbank()
                        for g in range(8):
                            pm, pmB = (pm0, pm0B) if g < 4 else (pm1, pm1B)
                            mm(pm[:, (g % 4) * 128:(g % 4 + 1) * 128], vn[:, g * 128:(g + 1) * 128], wsT[:, g, :],
                               True, True, reads=[vnB, CB], writes=[pmB])
                        for hh, (pm, pmB) in enumerate(((pm0, pm0B), (pm1, pm1B))):
                            tt(tmpm[:, hh * 4:(hh + 1) * 4, :], pm[:].rearrange("p (g q) -> p g q", q=128),
                               bs_bc[:, hh * 4:(hh + 1) * 4, :], ALU.add, reads=[pmB, CB], writes=[tmpmB])
                        tt(sguT[:, :, tl * 128:(tl + 1) * 128], sguT[:, :, tl * 128:(tl + 1) * 128], tmpm[:], ALU.mult,
                           reads=[tmpmB, sguB], writes=[sguB])
                    if dbg:
                        final_ops.append(dma(SGd[:, :, c * TC:(c + 1) * TC], sguT[:], reads=[sguB]))
                    for blk in range(D // WBK):
                        wga, wgaB = wl(wblk, win_v[:, :, C_GA + blk * WBK:C_GA + (blk + 1) * WBK])
                        wgg, wggB = wl(wblk, win_v[:, :, C_GG + blk * WBK:C_GG + (blk + 1) * WBK])
                        wpa, wpaB = wl(wblk2, wap_v[:, :, blk * WBK:(blk + 1) * WBK])
                        wpg, wpgB = wl(wblk2, wgp_v[:, :, blk * WBK:(blk + 1) * WBK])
                        for j in range(NJ):
                            dm = blk * NJ + j
                            js = slice(j * 128, (j + 1) * 128)
                            sigs = []
                            for (wg, wgB, col) in ((wga, wgaB, dm), (wgg, wggB, 16 + dm)):
                                pb, pbB_ = nextbank()
                                for dt in range(16):
                                    mm(pb[:], wg[:, dt, js], xsTc[:, dt, :], dt == 0, dt == 15,
                                       reads=[wgB] + xsTcB, writes=[pbB_])
                                sg_, sgB = sig_r.next()
                                act(sg_[:], pb[:], AF.Sigmoid, reads=[pbB_, CB], writes=[sgB], bias=bgT[:, col:col + 1])
                                sigs.append((sg_, sgB))
                            ms = []
                            for k, (wp, wpB) in enumerate(((wpa, wpaB), (wpg, wpgB))):
                                pb, pbB_ = nextbank()
                                for kt in range(8):
                                    if k == 0:
                                        rhs = QA[:, kt, c * TC:(c + 1) * TC]
                                        rds = [wpB] + [QAB[kt // 4][c * NTC + u] for u in range(NTC)]
                                    else:
                                        rhs = sguT[:, kt, :]
                                        rds = [wpB, sguB]
                                    mm(pb[:], wp[:, kt, js], rhs, kt == 0, kt == 7, reads=rds, writes=[pbB_])
                                m1, m1B = m1_r.next()
                                tt(m1[:], sigs[k][0][:], pb[:], ALU.mult, reads=[sigs[k][1], pbB_], writes=[m1B])
                                ms.append((m1, m1B))
                            tt(mT[:, dm, :], ms[0][0][:], ms[1][0][:], ALU.add, reads=[ms[0][1], ms[1][1]],
                               writes=[mTB[dm]])
                    if dbg:
                        final_ops.append(dma(MTd[:, :, c * TC:(c + 1) * TC], mT[:], reads=mTB))
                    steps4 = [(cb, tl) for cb in range(D // WBK) for tl in range(NTC)]
                    xrq = {}

                    def xr_load(k):
                        cb, tl = steps4[k]
                        row = c * TC + tl * 128
                        xr, xrB = xr_r.next()
                        dma(xr[:], x_own[row:row + 128, cb * WBK:(cb + 1) * WBK], writes=[xrB])
                        xrq[k] = (xr, xrB)

                    for k in range(min(2, len(steps4))):
                        xr_load(k)
                    wo = woB = None
                    for k, (cb, tl) in enumerate(steps4):
                        if tl == 0:
                            wo, woB = wl(wblk, wout_v[:, :, cb * WBK:(cb + 1) * WBK])
                        if k + 2 < len(steps4):
                            xr_load(k + 2)
                        row = c * TC + tl * 128
                        xr, xrB = xrq.pop(k)
                        pb, pbB_ = nextbank()
                        for dm in range(16):
                            mm(pb[:, 0:WBK], mT[:, dm, tl * 128:(tl + 1) * 128], wo[:, dm, :], dm == 0, dm == 15,
                               reads=[woB, mTB[dm]], writes=[pbB_])
                        h2t, h2B_ = h2_r.next()
                        tt(h2t[:], xr[:], pb[:, 0:WBK], ALU.add, reads=[xrB, pbB_], writes=[h2B_])
                        o = dma(H2[row:row + 128, cb * WBK:(cb + 1) * WBK], h2t[:], reads=[h2B_],
                                writes=[H2B[c * NTC + tl]])
                        if dbg:
                            final_ops.append(o)
            P.barrier()

        esQA.close()
        if "E" in phases:
            with ExitStack() as es:
                def sb(name, shape, dt):
                    return es.enter_context(nc.sbuf_tensor("E_" + name, list(shape), dt))

                def ps(name, shape, dt):
                    return es.enter_context(nc.psum_tensor("E_" + name, list(shape), dt))

                NTC = TC // 128
                AG = 64
                NAG = 128 // AG
                KxT = sb("KxT", [128, 16, 128], F32)
                with ExitStack() as es2:
                    ktmp = es2.enter_context(nc.sbuf_tensor("ktmp", [128, 16, 128], F32))
                    pk_ = es2.enter_context(nc.psum_tensor("pk_", [128, 16, 128], F32))
                    b = Buf()
                    kview = ktmp[:].rearrange("p (h two) d -> p h two d", two=2)
                    dma(kview[:, :, 0, :], pk1.rearrange("h n d -> n h d"), writes=[b])
                    dma(kview[:, :, 1, :], pk2.rearrange("h n d -> n h d"), writes=[b])
                    for j in range(16):
                        tr(pk_[:, j, :], ktmp[:, j, :], ident_f[:], reads=[b, CB], writes=[b])
                    cp(KxT[:], pk_[:], reads=[b], writes=[CB])
                    P.barrier()
                R = {
                    "st": Ring(sb, "st", [128, 4], F32, 2),
                    "xs": Ring(sb, "xs", [128, D], BF16, 1),
                    "pT": Ring(ps, "pT", [128, 16, 128], BF16, 2),
                }
                Gt = sb("Gt", [128, AG * TC], BF16)
                G3 = Gt[:].rearrange("p (a t) -> p a t", t=TC)
                qpT = Gt[:].bitcast(F32).rearrange("p (j t) -> p j t", t=TC)
                GB = Buf("G")
                oacc = sb("oacc", [128, NTC, D], F32)
                oaccB = [Buf() for _ in range(NTC)]
                xn2T = sb("xn2T", [128, 16, TC], BF16)
                xn2B = [Buf() for _ in range(NTC)]
                rT = sb("rT", [128, 3, TC], F32)
                rTB = Buf()
                wpq_r = Ring(sb, "wpq", [128, 16, 128], BF16, 2)
                ub_r = Ring(sb, "ub", [128, D], BF16, 3)
                uT_r = Ring(sb, "uT", [128, 16, 128], BF16, 2)
                vb_r = Ring(sb, "vblk", [128, 512], BF16, 6)
                hb_r = Ring(sb, "hb", [128, TC], BF16, 2)
                ohb_r = Ring(sb, "ohb", [128, 8, 128], BF16, 2)
                oha_r = Ring(sb, "oha", [128, 8, AG], BF16, 3)
                vv = sb("vv", [128, 16, 16], F32)
                vi = sb("vi", [128, 16, 16], U32)
                vif = sb("vif", [128, 16, 16], F32)
                wk = sb("wk", [128, 16, 128], F32)
                cand = sb("cand", [128, 8, 256], F32)
                cwk = sb("cwk", [128, 8, 256], F32)
                cv = sb("cv", [128, 8, 16], F32)
                ci = sb("ci", [128, 8, 16], U32)
                cih = sb("cih", [128, 8, 16], U32)
                cil = sb("cil", [128, 8, 16], U32)
                cihf = sb("cihf", [128, 8, 16], F32)
                cilf = sb("cilf", [128, 8, 16], F32)
                abg = sb("abg", [128, 3, 128], F32)
                zz = sb("zz", [128, 16], F32)
                RB = Buf("route")
                L1B = [Buf() for _ in range(16)]
                L2B = [Buf() for _ in range(8)]
                w_pq_v = WPQb.rearrange("(dt p) n -> p dt n", p=128)
                pb4 = [ps("pe%d" % k, [128, 512], F32) for k in range(4)]
                pb4B = [Buf() for _ in range(4)]
                pei = [0]

                def nextbank4(lo=0, hi=4):
                    k = lo + pei[0] % (hi - lo)
                    pei[0] += 1
                    return pb4[k], pb4B[k]

                for c in range(NCH):
                    for tl in range(NTC):
                        row = c * TC + tl * 128
                        dma(oacc[:, tl, :], H2[row:row + 128, :], reads=[H2B[c * NTC + tl]], writes=[oaccB[tl]])

                    for tl in range(NTC):
                        norm_T_sb(oacc[:, tl, :], oaccB[tl], gffnT, xn2T[:, :, tl * 128:(tl + 1) * 128], xn2B[tl], R)
                    for j in range(16):
                        wb, wbB = wpq_r.next()
                        dma(wb[:], w_pq_v[:, :, j * 128:(j + 1) * 128], reads=[WcB], writes=[wbB], q=POOL)
                        pb, pbB_ = nextbank4()
                        for dt in range(16):
                            mm(pb[:], wb[:, dt, :], xn2T[:, dt, :], dt == 0, dt == 15, reads=[wbB] + xn2B, writes=[pbB_])
                        act(qpT[:, j, :], pb[:], AF.Copy, reads=[pbB_], writes=[GB])
                    for tl in range(NTC):
                        tsl = slice(tl * 128, (tl + 1) * 128)
                        sbk = []
                        for q4 in range(4):
                            pb, pbB_ = nextbank4(0, 4)
                            sbk.append((pb, pbB_))
                            for jj in range(4):
                                j = q4 * 4 + jj
                                mm(pb[:, jj * 128:(jj + 1) * 128], qpT[:, j, tsl], KxT[:, j, :], True, True,
                                   reads=[GB, CB], writes=[pbB_])

                        def sc(j):
                            return sbk[j // 4][0][:, (j % 4) * 128:(j % 4 + 1) * 128], sbk[j // 4][1]
                        for j in range(16):
                            s_, sB = sc(j)
                            P.add(DVE, lambda e, j=j, s_=s_: e.max(out=vv[:, j, 0:8], in_=s_), reads=[sB], writes=[L1B[j]])
                        for j in range(16):
                            s_, sB = sc(j)
                            P.add(DVE, lambda e, j=j, s_=s_: e.max_index(out=vi[:, j, 0:8], in_max=vv[:, j, 0:8], in_values=s_),
                                  reads=[sB, L1B[j]], writes=[L1B[j]])
                        for j in range(16):
                            s_, sB = sc(j)
                            P.add(DVE, lambda e, j=j, s_=s_: e.match_replace(out=wk[:, j, :], in_to_replace=vv[:, j, 0:8],
                                                                            in_values=s_, imm_value=-1e30),
                                  reads=[sB, L1B[j]], writes=[L1B[j]])
                        for j in range(16):
                            P.add(DVE, lambda e, j=j: e.max(out=vv[:, j, 8:16], in_=wk[:, j, :]), reads=[L1B[j]], writes=[L1B[j]])
                        for j in range(16):
                            P.add(DVE, lambda e, j=j: e.max_index(out=vi[:, j, 8:16], in_max=vv[:, j, 8:16], in_values=wk[:, j, :]),
                                  reads=[L1B[j]], writes=[L1B[j]])
                        cp(vif[:], vi[:], reads=L1B, writes=[RB])
                        tt(cand[:].rearrange("p h (i j) -> p h i j", j=16),
                           cap(vv[:, 0, :], [(32, 8), (1, 16), (0, 16)]),
                           cap(vv[:, 1, :], [(32, 8), (0, 16), (1, 16)]), ALU.add, reads=L1B, writes=[RB])
                        for h in range(8):
                            P.add(DVE, lambda e, h=h: e.max(out=cv[:, h, 0:8], in_=cand[:, h, :]), reads=[RB], writes=[L2B[h]])
                        for h in range(8):
                            P.add(DVE, lambda e, h=h: e.max_index(out=ci[:, h, 0:8], in_max=cv[:, h, 0:8], in_values=cand[:, h, :]),
                                  reads=[RB, L2B[h]], writes=[L2B[h]])
                        for h in range(8):
                            P.add(DVE, lambda e, h=h: e.match_replace(out=cwk[:, h, :], in_to_replace=cv[:, h, 0:8],
                                                                      in_values=cand[:, h, :], imm_value=-1e30),
                                  reads=[RB, L2B[h]], writes=[L2B[h]])
                        for h in range(8):
                            P.add(DVE, lambda e, h=h: e.max(out=cv[:, h, 8:16], in_=cwk[:, h, :]), reads=[L2B[h]], writes=[L2B[h]])
                        for h in range(8):
                            P.add(DVE, lambda e, h=h: e.max_index(out=ci[:, h, 8:16], in_max=cv[:, h, 8:16], in_values=cwk[:, h, :]),
                                  reads=[L2B[h]], writes=[L2B[h]])
                        P.add(DVE, lambda e: e.tensor_single_scalar(out=cih[:], in_=ci[:], scalar=4, op=ALU.logical_shift_right),
                              reads=L2B, writes=[RB])
                        P.add(DVE, lambda e: e.tensor_single_scalar(out=cil[:], in_=ci[:], scalar=15, op=ALU.bitwise_and),
                              reads=L2B, writes=[RB])
                        cp(cihf[:], cih[:], reads=[RB], writes=[RB])
                        cp(cilf[:], cil[:], reads=[RB], writes=[RB])
                        oh = wk[:].rearrange("p j n -> p (j n)")
                        oh4 = oh.rearrange("p (h k i) -> p h k i", k=16, i=16)
                        for which, (sel, tab) in enumerate(((cihf, 0), (cilf, 1))):
                            tt(oh4, cap(iota_f[:, 0:16], [(0, 8), (0, 16), (1, 16)]),
                               cap(sel[:, 0, :], [(16, 8), (1, 16), (0, 16)]), ALU.is_equal, reads=[RB, CB], writes=[RB] + L1B)
                            tt(oh4, oh4, cap(vif[:, tab, :], [(32, 8), (0, 16), (1, 16)]), ALU.mult, reads=[RB], writes=[RB] + L1B)
                            red(abg[:, which, :], oh.rearrange("p (hk i) -> p hk i", i=16), reads=[RB] + L1B, writes=[RB])
                        tt(cwk[:, :, 0:16], cv[:], cap(cv[:, 0, 0:1], [(16, 8), (0, 16)]), ALU.subtract, reads=L2B, writes=[RB] + L2B)
                        act(cwk[:, :, 0:16], cwk[:, :, 0:16], AF.Exp, reads=[RB], writes=[RB] + L2B)
                        red(zz[:, 0:8], cwk[:, :, 0:16], reads=[RB], writes=[RB])
                        rcp(zz[:, 8:16], zz[:, 0:8], reads=[RB], writes=[RB])
                        tt(abg[:, 2, :].rearrange("p (h k) -> p h k", k=16), cwk[:, :, 0:16],
                           cap(zz[:, 8:16], [(1, 8), (0, 16)]), ALU.mult, reads=[RB], writes=[RB])
                        pTt, pbB_ = R["pT"].next()
                        pb = pTt[:].rearrange("p a b -> p (a b)").bitcast(F32)
                        for w3 in range(3):
                            tr(pb[:, w3 * 128:(w3 + 1) * 128], abg[:, w3, :], ident_f[:], reads=[RB, CB], writes=[pbB_])
                        cp(rT[:, :, tsl], pb[:, 0:384].rearrange("p (w t) -> p w t", t=128), reads=[pbB_], writes=[rTB])
                    if dbg:
                        final_ops.append(dma(RTd[:, :, c * TC:(c + 1) * TC].rearrange("w p t -> p w t"), rT[:], reads=[rTB]))
                    P.barrier()
                    for ag in range(NAG):
                        ioT, ioB = R["pT"].next()
                        ioT2, ioB2 = R["pT"].next()
                        io_b = ioT[:].rearrange("p a b -> p (a b)").bitcast(F32).rearrange("p (t b) -> p t b", b=128)
                        io_a = ioT2[:].rearrange("p a b -> p (a b)").bitcast(F32)[:, 0:8 * AG].rearrange("p (t a) -> p t a", a=AG)
                        cp(io_b, cap(iota_f[:, 0:128], [(0, 8), (1, 128)]), reads=[CB], writes=[ioB])
                        cp(io_a, cap(iota_f[:, ag * AG:(ag + 1) * AG], [(0, 8), (1, AG)]), reads=[CB], writes=[ioB2])
                        for t0 in range(0, TC, 8):
                            ohb, ohbB = ohb_r.next()
                            oha, ohaB = oha_r.next()
                            pb, pbB_ = nextbank4(0, 4)
                            tt(ohb[:], io_b, cap(rT[:, 1, t0:t0 + 8], [(1, 8), (0, 128)]), ALU.is_equal,
                               reads=[rTB, ioB], writes=[ohbB])
                            tt(oha[:], io_a, cap(rT[:, 0, t0:t0 + 8], [(1, 8), (0, AG)]), ALU.is_equal,
                               reads=[rTB, ioB2], writes=[ohaB])
                            tt(oha[:], oha[:], cap(rT[:, 2, t0:t0 + 8], [(1, 8), (0, AG)]), ALU.mult,
                               reads=[rTB], writes=[ohaB], eng=POOL)
                            for u in range(8):
                                mm(pb[:, u * AG:(u + 1) * AG], ohb[:, u, :], oha[:, u, :], True, True,
                                   reads=[ohbB, ohaB], writes=[pbB_])
                            act(G3[:, :, t0:t0 + 8], pb[:, 0:8 * AG].rearrange("p (t a) -> p a t", a=AG), AF.Copy,
                                reads=[pbB_], writes=[GB])
                        def e2_front(al):
                            a = ag * AG + al
                            ub, ubB = ub_r.next()
                            dma(ub[:], UB16[a * 128:(a + 1) * 128, :], reads=[UcB[a // 4]], writes=[ubB], q=POOL)
                            pT, pTB = R["pT"].next()
                            for dt in range(16):
                                tr(pT[:, dt, :], ub[:, dt * 128:(dt + 1) * 128], ident_b[:], reads=[ubB, CB], writes=[pTB])
                            uT, uTB = uT_r.next()
                            cp(uT[:], pT[:], reads=[pTB], writes=[uTB])
                            return uT, uTB

                        fr = e2_front(0)
                        for al in range(AG):
                            nfr = e2_front(al + 1) if al + 1 < AG else None
                            uT, uTB = fr
                            pb, pbB_ = nextbank4(0, 2)
                            for dt in range(16):
                                mm(pb[:], uT[:, dt, :], xn2T[:, dt, :], dt == 0, dt == 15, reads=[uTB] + xn2B, writes=[pbB_])
                            hb, hbB = hb_r.next()
                            act(hb[:], pb[:], AF.Gelu_apprx_tanh, reads=[pbB_], writes=[hbB])
                            tt(G3[:, al, :], hb[:], G3[:, al, :], ALU.mult, reads=[hbB, GB], writes=[GB])
                            fr = nfr
                        for db in range(4):
                            banks = [(pb4[tl], pb4B[tl]) for tl in range(NTC)]
                            for al in range(AG):
                                a = ag * AG + al
                                vb, vbB = vb_r.next()
                                dma(vb[:], VB16[a * 128:(a + 1) * 128, db * 512:(db + 1) * 512], reads=[VcB[a // 4]], writes=[vbB])
                                for tl in range(NTC):
                                    mm(banks[tl][0][:], G3[:, al, tl * 128:(tl + 1) * 128], vb[:], al == 0, al == AG - 1,
                                       reads=[GB, vbB], writes=[banks[tl][1]])
                            for tl in range(NTC):
                                osl = oacc[:, tl, db * 512:(db + 1) * 512]
                                tt(osl, osl, banks[tl][0][:], ALU.add, reads=[banks[tl][1]], writes=[oaccB[tl]])
                    for tl in range(NTC):
                        row = c * TC + tl * 128
                        final_ops.append(dma(out[row:row + 128, :], oacc[:, tl, :], reads=[oaccB[tl]]))
            P.barrier()

        if not final_ops:
            final_ops.append(P.all_ops[-1])
        P.prepare(eng_sems, dma_sems, final_ops)
        block = es0.enter_context(nc.Block())
        for nm, en in (("sync", SP), ("tensor", PE), ("scalar", ACT), ("vector", DVE), ("gpsimd", POOL)):
            getattr(block, nm)(lambda e, en=en: P.emit(en, e))
    return nc


def rope_cs(S):
    rows = S // 64
    row = np.repeat(np.arange(rows, dtype=np.float32), 64)
    col = np.tile(np.arange(64, dtype=np.float32), rows)
    inv = (np.float32(10000.0) ** (-np.arange(32, dtype=np.float32) / np.float32(32))).astype(np.float32)
    ang = np.concatenate([row[:, None] * inv, col[:, None] * inv], axis=-1).astype(np.float32)
    return np.concatenate([np.cos(ang), np.sin(ang)], axis=-1).astype(np.float32)


def make_in_maps(inputs, S, NC):
    TOK = S // NC
    f = lambda a: np.ascontiguousarray(np.asarray(a, dtype=np.float32))
    x = f(inputs["x"]).reshape(-1, D)[:S]
    cs = rope_cs(S)
    shared = {
        "x": x, "cs": cs,
        "norm_mix": f(inputs["norm_mix"]).reshape(16, 128),
        "w_in": f(inputs["w_in"]).reshape(D, 7680),
        "b_gate": f(inputs["b_gate"]).reshape(32, 128),
        "q_norm": f(inputs["q_norm"]).reshape(1, 128),
        "k_norm": f(inputs["k_norm"]).reshape(1, 128),
        "sgu_norm": f(inputs["sgu_norm"]).reshape(1, 1024),
        "w_sgu": f(inputs["w_sgu"]).reshape(8, 128, 128),
        "b_sgu": f(inputs["b_sgu"]).reshape(1, 1024),
        "w_attn_proj": f(inputs["w_attn_proj"]).reshape(1024, D),
        "w_sgu_proj": f(inputs["w_sgu_proj"]).reshape(1024, D),
        "w_out": f(inputs["w_out"]).reshape(D, D),
        "norm_ffn": f(inputs["norm_ffn"]).reshape(16, 128),
        "w_peer_q": f(inputs["w_peer_q"]).reshape(D, D),
        "peer_k1": f(inputs["peer_k1"]).reshape(8, 128, 128),
        "peer_k2": f(inputs["peer_k2"]).reshape(8, 128, 128),
        "peer_u": f(inputs["peer_u"]).reshape(NEXP, D),
        "peer_v": f(inputs["peer_v"]).reshape(NEXP, D),
    }
    maps = []
    for c in range(NC):
        m = dict(shared)
        m["x_own"] = np.ascontiguousarray(x[c * TOK:(c + 1) * TOK])
        m["cs_own"] = np.ascontiguousarray(cs[c * TOK:(c + 1) * TOK])
        maps.append(m)
    return maps


_NC_CACHE = {}


def kernel(**inputs):
    S, NC = 16384, 8
    if "nc" not in _NC_CACHE:
        _NC_CACHE["nc"] = build(S, NC)
    nc = _NC_CACHE["nc"]
    in_maps = make_in_maps(inputs, S, NC)
    res = run_bass_kernel_spmd(nc, in_maps, core_ids=list(range(NC)))
    outs = [np.asarray(r["out"], dtype=np.float32) for r in res.results]
    return np.concatenate(outs, axis=0).reshape(1, S, D)
```

```python
import numpy as np
from contextlib import ExitStack
import concourse.bass as bass
import concourse.mybir as mybir
from concourse.bass_utils import run_bass_kernel_spmd

F32 = mybir.dt.float32
BF16 = mybir.dt.bfloat16
U32 = mybir.dt.uint32
AF = mybir.ActivationFunctionType
ALU = mybir.AluOpType
AX = mybir.AxisListType

PE, ACT, DVE, POOL, SP = "tensor", "scalar", "vector", "gpsimd", "sync"
ENGINES = (PE, ACT, DVE, POOL, SP)
N_DMA_SEMS = 48

D = 2048
EPS = 1e-6
C_Q, C_K, C_V, C_SU, C_SV, C_GA, C_GG = 0, 1024, 1280, 1536, 2560, 3584, 5632
NEXP = 16384


class Buf:
    __slots__ = ("name", "last_write", "readers")

    def __init__(self, name=""):
        self.name = name
        self.last_write = None
        self.readers = []


class Op:
    __slots__ = ("eng", "fn", "deps", "is_dma", "signal", "sem", "val")

    def __init__(self, eng, fn, is_dma):
        self.eng = eng
        self.fn = fn
        self.deps = []
        self.is_dma = is_dma
        self.signal = False
        self.sem = None
        self.val = None


class Prog:
    def __init__(self):
        self.ops = {e: [] for e in ENGINES}
        self.all_ops = []
        self.pending = {e: None for e in ENGINES}
        self.dma_since = []

    def add(self, eng, fn, reads=(), writes=(), deps=(), dma=False):
        op = Op(eng, fn, dma)
        ds = []
        for b in reads:
            if b.last_write is not None:
                ds.append(b.last_write)
        for b in writes:
            if b.last_write is not None:
                ds.append(b.last_write)
            ds.extend(b.readers)
        ds.extend(d for d in deps if d is not None)
        if self.pending[eng] is not None:
            ds.extend(self.pending[eng])
            self.pending[eng] = None
        seen = set()
        for d in ds:
            if id(d) in seen:
                continue
            seen.add(id(d))
            if (not d.is_dma) and (not dma) and d.eng == eng and eng == PE:
                continue
            op.deps.append(d)
            d.signal = True
        for b in reads:
            b.readers.append(op)
        for b in writes:
            b.last_write = op
            b.readers = []
        self.ops[eng].append(op)
        self.all_ops.append(op)
        if dma:
            self.dma_since.append(op)
        return op

    def barrier(self):
        last = []
        for e in ENGINES:
            for o in reversed(self.ops[e]):
                if not o.is_dma:
                    last.append(o)
                    break
        last.extend(self.dma_since)
        self.dma_since = []
        for e in ENGINES:
            self.pending[e] = list(last)

    def prepare(self, eng_sems, dma_sems, final_ops):
        for o in final_ops:
            o.signal = True
        cnt = {e: 0 for e in ENGINES}
        dma_tot = [0] * len(dma_sems)
        dma_last = [None] * len(dma_sems)
        n_all = len(dma_sems)
        pools = {SP: (0, n_all // 2), ACT: (n_all // 2, n_all // 2 + n_all // 8),
                 POOL: (n_all // 2 + n_all // 8, n_all)}
        rrq = {SP: 0, ACT: 0, POOL: 0}
        for op in self.all_ops:
            if op.is_dma:
                lo, hi = pools[op.eng]
                k = lo + rrq[op.eng] % (hi - lo)
                rrq[op.eng] += 1
                if dma_last[k] is not None:
                    op.deps.append(dma_last[k])
                dma_tot[k] += 16
                op.sem = dma_sems[k]
                op.val = dma_tot[k]
                dma_last[k] = op
            elif op.signal:
                cnt[op.eng] += 1
                op.sem = eng_sems[op.eng]
                op.val = cnt[op.eng]
        self.final_ops = final_ops

    def emit(self, eng, e):
        waited = {}
        for op in self.ops[eng]:
            need = {}
            for d in op.deps:
                key = id(d.sem)
                if key not in need or need[key][1] < d.val:
                    need[key] = (d.sem, d.val)
            for key, (sem, val) in need.items():
                if waited.get(key, 0) >= val:
                    continue
                e.wait_ge(sem, val)
                waited[key] = val
            ins = op.fn(e)
            if op.is_dma:
                ins.then_inc(op.sem, 16)
            elif op.signal:
                ins.then_inc(op.sem, 1)
        if eng == SP:
            for o in self.final_ops:
                key = id(o.sem)
                if waited.get(key, 0) < o.val:
                    e.wait_ge(o.sem, o.val)
                    waited[key] = o.val


class Ring:
    def __init__(self, alloc, name, shape, dt, n):
        self.tiles = [alloc("%s%d" % (name, i), shape, dt) for i in range(n)]
        self.bufs = [Buf("%s%d" % (name, i)) for i in range(n)]
        self.i = 0

    def next(self):
        k = self.i % len(self.tiles)
        self.i += 1
        return self.tiles[k], self.bufs[k]


def cap(ap, dims):
    return bass.AP(ap.tensor, ap.offset, [list(ap.ap[0])] + [[s, n] for s, n in dims])


def build(S, NC, dbg=False, phases="ABCE"):
    NT_ALL = S // 128
    TOK = S // NC
    NT_OWN = TOK // 128
    TC = 512
    NCH = TOK // TC
    nc = bass.Bass("TRN2", target_bir_lowering=False)
    P = Prog()

    def din(name, shape, dt=F32):
        return nc.dram_tensor(name, list(shape), dt, kind="ExternalInput").ap()

    x_all = din("x", [S, D])
    x_own = din("x_own", [TOK, D])
    cs_all = din("cs", [S, 128])
    cs_own = din("cs_own", [TOK, 128])
    norm_mix = din("norm_mix", [16, 128])
    w_in = din("w_in", [D, 7680])
    b_gate = din("b_gate", [32, 128])
    q_norm = din("q_norm", [1, 128])
    k_norm = din("k_norm", [1, 128])
    sgu_norm = din("sgu_norm", [1, 1024])
    w_sgu = din("w_sgu", [8, 128, 128])
    b_sgu = din("b_sgu", [1, 1024])
    w_ap = din("w_attn_proj", [1024, D])
    w_gp = din("w_sgu_proj", [1024, D])
    w_out = din("w_out", [D, D])
    norm_ffn = din("norm_ffn", [16, 128])
    w_pq = din("w_peer_q", [D, D])
    pk1 = din("peer_k1", [8, 128, 128])
    pk2 = din("peer_k2", [8, 128, 128])
    peer_u = din("peer_u", [NEXP, D])
    peer_v = din("peer_v", [NEXP, D])
    out = nc.dram_tensor("out", [TOK, D], F32, kind="ExternalOutput").ap()
    skind = "ExternalOutput" if dbg else "Internal"
    KT = nc.dram_tensor("KT", [2, 128, S], BF16, kind=skind).ap()
    VD = nc.dram_tensor("VD", [2, S, 128], BF16, kind=skind).ap()
    H2 = nc.dram_tensor("H2", [TOK, D], F32, kind=skind).ap()
    WINb = nc.dram_tensor("WINb", [D, 7680], BF16, kind="Internal").ap()
    WAPb = nc.dram_tensor("WAPb", [1024, D], BF16, kind="Internal").ap()
    WGPb = nc.dram_tensor("WGPb", [1024, D], BF16, kind="Internal").ap()
    WOUTb = nc.dram_tensor("WOUTb", [D, D], BF16, kind="Internal").ap()
    WPQb = nc.dram_tensor("WPQb", [D, D], BF16, kind="Internal").ap()
    UB16 = nc.dram_tensor("UB16", [NEXP, D], BF16, kind="Internal").ap()
    VB16 = nc.dram_tensor("VB16", [NEXP, D], BF16, kind="Internal").ap()
    WcB = Buf("wcast")
    UcB = [Buf() for _ in range(NEXP // 512)]
    VcB = [Buf() for _ in range(NEXP // 512)]
    if dbg:
        QAd = nc.dram_tensor("QAd", [128, 8, TOK], BF16, kind="ExternalOutput").ap()
        ATd = nc.dram_tensor("ATd", [128, 8, TOK], BF16, kind="ExternalOutput").ap()
        SGd = nc.dram_tensor("SGd", [128, 8, TOK], BF16, kind="ExternalOutput").ap()
        MTd = nc.dram_tensor("MTd", [128, 16, TOK], BF16, kind="ExternalOutput").ap()
        RTd = nc.dram_tensor("RTd", [3, 128, TOK], F32, kind="ExternalOutput").ap()
    KTB = [[Buf() for _ in range(NT_ALL)] for _ in range(2)]
    VDB = [[Buf() for _ in range(NT_ALL)] for _ in range(2)]
    H2B = [Buf() for _ in range(NT_OWN)]
    final_ops = []

    with ExitStack() as es0:
        def sb0(name, shape, dt):
            return es0.enter_context(nc.sbuf_tensor(name, list(shape), dt))

        eng_sems = {e: es0.enter_context(nc.semaphore("s_" + e)) for e in ENGINES}
        dma_sems = [es0.enter_context(nc.semaphore("d%d" % i)) for i in range(N_DMA_SEMS)]

        def dma(o, i, reads=(), writes=(), q=SP):
            return P.add(q, lambda e: e.dma_start(out=o, in_=i), reads, writes, dma=True)

        def mm(o, lhsT, rhs, start, stop, reads=(), writes=()):
            return P.add(PE, lambda e: e.matmul(o, lhsT=lhsT, rhs=rhs, start=start, stop=stop), reads, writes)

        def tr(o, i, ident, reads=(), writes=()):
            return P.add(PE, lambda e: e.transpose(out=o, in_=i, identity=ident), reads, writes)

        def act(o, i, func, reads=(), writes=(), **kw):
            return P.add(ACT, lambda e: e.activation(out=o, in_=i, func=func, **kw), reads, writes)

        def tt(o, a, b, op, reads=(), writes=(), eng=DVE):
            return P.add(eng, lambda e: e.tensor_tensor(out=o, in0=a, in1=b, op=op), reads, writes)

        def ts(o, a, s1, s2, op0, op1=None, reads=(), writes=(), eng=DVE):
            if op1 is None:
                return P.add(eng, lambda e: e.tensor_scalar(out=o, in0=a, scalar1=s1, scalar2=None, op0=op0), reads, writes)
            return P.add(eng, lambda e: e.tensor_scalar(out=o, in0=a, scalar1=s1, scalar2=s2, op0=op0, op1=op1), reads, writes)

        def cp(o, i, reads=(), writes=(), eng=DVE):
            return P.add(eng, lambda e: e.tensor_copy(out=o, in_=i), reads, writes)

        def red(o, i, reads=(), writes=()):
            return P.add(DVE, lambda e: e.tensor_reduce(out=o, in_=i, axis=AX.X, op=ALU.add), reads, writes)

        def rcp(o, i, reads=(), writes=()):
            return P.add(DVE, lambda e: e.reciprocal(out=o, in_=i), reads, writes)

        ident_f = sb0("ident_f", [128, 128], F32)
        ident_b = sb0("ident_b", [128, 128], BF16)
        iota_b = sb0("iota_b", [128, 128], BF16)
        iota_f = sb0("iota_f", [128, 128], F32)
        iota_p = sb0("iota_p", [128, 1], F32)
        gmixT = sb0("gmixT", [128, 16], F32)
        gffnT = sb0("gffnT", [128, 16], F32)
        CB = Buf("consts")
        P.add(POOL, lambda e: e.iota(iota_p[:], pattern=[[0, 1]], base=0, channel_multiplier=1,
                                     allow_small_or_imprecise_dtypes=True), writes=[CB])
        P.add(POOL, lambda e: e.iota(iota_f[:], pattern=[[1, 128]], base=0, channel_multiplier=0,
                                     allow_small_or_imprecise_dtypes=True), writes=[CB])
        ts(ident_f[:], iota_f[:], iota_p[:, 0:1], None, ALU.is_equal, reads=[CB], writes=[CB])
        cp(ident_b[:], ident_f[:], reads=[CB], writes=[CB])
        cp(iota_b[:], iota_f[:], reads=[CB], writes=[CB])

        esQA = ExitStack()
        QA = esQA.enter_context(nc.sbuf_tensor("QA", [128, 8, TOK], BF16))
        QAB = [[Buf() for _ in range(NT_OWN)] for _ in range(2)]

        def load_colvec(dst, src, rows, es):
            tmp = es.enter_context(nc.sbuf_tensor("cv_tmp_%s" % dst.name, [rows, 128], F32))
            ptmp = es.enter_context(nc.psum_tensor("cv_ps_%s" % dst.name, [128, rows], F32))
            b = Buf()
            dma(tmp[:], src, writes=[b])
            tr(ptmp[:], tmp[:], ident_f[0:rows, 0:rows], reads=[b, CB], writes=[b])
            cp(dst[:], ptmp[:], reads=[b], writes=[CB])

        with ExitStack() as es:
            load_colvec(gmixT, norm_mix, 16, es)
            load_colvec(gffnT, norm_ffn, 16, es)
        P.barrier()

        def norm_T(src, gT, dst, dstB, R):
            xt, xtB = R["xt"].next()
            dma(xt[:], src, writes=[xtB])
            return norm_T_sb(xt[:], xtB, gT, dst, dstB, R)

        def norm_T_sb(xt, xtB, gT, dst, dstB, R, defer=False, dstB2=None):
            st, stB = R["st"].next()
            xs, xsB = R["xs"].next()
            pT, pTB = R["pT"].next()
            act(xs[:], xt, AF.Square, reads=[xtB], writes=[xsB, stB], accum_out=st[:, 0:1])
            act(st[:, 1:2], st[:, 0:1], AF.Sqrt, reads=[stB], writes=[stB], scale=1.0 / D, bias=EPS)
            rcp(st[:, 2:3], st[:, 1:2], reads=[stB], writes=[stB])
            if defer:
                act(xs[:], xt, AF.Copy, reads=[xtB], writes=[xsB])
            else:
                act(xs[:], xt, AF.Copy, reads=[xtB, stB], writes=[xsB], scale=st[:, 2:3])
            for dt in range(16):
                tr(pT[:, dt, :], xs[:, dt * 128:(dt + 1) * 128], ident_b[:], reads=[xsB, CB], writes=[pTB])
            if dstB2 is None:
                tt(dst, pT[:], cap(gT[:], [(1, 16), (0, 128)]), ALU.mult, reads=[pTB, CB], writes=[dstB])
            else:
                tt(dst[:, 0:8, :], pT[:, 0:8, :], cap(gT[:, 0:8], [(1, 8), (0, 128)]), ALU.mult,
                   reads=[pTB, CB], writes=[dstB])
                tt(dst[:, 8:16, :], pT[:, 8:16, :], cap(gT[:, 8:16], [(1, 8), (0, 128)]), ALU.mult,
                   reads=[pTB, CB], writes=[dstB2])
            return st, stB

        def qk_norm_rope(src, srcB, nh, gain, cst, cstB, kr, krB, R):
            W = nh * 128
            sq, kn, t1, t2, sm = R["sq"], R["kn"], R["t1"], R["t2"], R["sm"]
            TB = R["ropeB"]
            src3 = src.rearrange("p (h d) -> p h d", d=128)
            act(sq[:, 0:W], src, AF.Square, reads=[srcB], writes=[TB])
            red(sm[:, 0:nh], sq[:, 0:W].rearrange("p (h d) -> p h d", d=128), reads=[TB], writes=[TB])
            act(sm[:, 16:16 + nh], sm[:, 0:nh], AF.Sqrt, reads=[TB], writes=[TB], scale=1.0 / 128, bias=EPS)
            rcp(sm[:, 32:32 + nh], sm[:, 16:16 + nh], reads=[TB], writes=[TB])
            kn3 = kn[:, 0:W].rearrange("p (h d) -> p h d", d=128)
            tt(kn3, src3, cap(sm[:, 32:32 + nh], [(1, nh), (0, 128)]), ALU.mult, reads=[srcB, TB], writes=[TB])
            tt(kn3, kn3, gain, ALU.mult, reads=[TB, CB], writes=[TB])
            x1 = kn[:, 0:W:2].rearrange("p (h j) -> p h j", j=64)
            x2 = kn[:, 1:W:2].rearrange("p (h j) -> p h j", j=64)
            c = cap(cst[:, 0:64], [(0, nh), (1, 64)])
            s = cap(cst[:, 64:128], [(0, nh), (1, 64)])
            a1 = t1[:, 0:nh * 64].rearrange("p (h j) -> p h j", j=64)
            a2 = t2[:, 0:nh * 64].rearrange("p (h j) -> p h j", j=64)
            tt(a1, x1, c, ALU.mult, reads=[TB, cstB], writes=[TB])
            tt(a2, x2, s, ALU.mult, reads=[TB, cstB], writes=[TB])
            tt(kr[:, :, 0:128:2], a1, a2, ALU.subtract, reads=[TB], writes=[krB])
            TB2 = R["ropeB2"]
            b1 = R["t3"][:, 0:nh * 64].rearrange("p (h j) -> p h j", j=64)
            b2 = R["t4"][:, 0:nh * 64].rearrange("p (h j) -> p h j", j=64)
            tt(b1, x1, s, ALU.mult, reads=[TB, cstB], writes=[TB2], eng=POOL)
            tt(b2, x2, c, ALU.mult, reads=[TB, cstB], writes=[TB2], eng=POOL)
            tt(kr[:, :, 1:128:2], b1, b2, ALU.add, reads=[TB2], writes=[krB], eng=POOL)

        def wload(dst, src, writes, nsplit=4):
            k = dst.shape[1]
            step = max(1, k // nsplit)
            ops = []
            for a in range(0, k, step):
                ops.append(dma(dst[:, a:a + step, :], src[:, a:a + step, :], writes=writes, q=POOL))
            return ops

        def emit_precasts(deps):
            def precast(dst, src, rows, cols, writes):
                k = 1920 if cols == 7680 else cols
                sv = src.rearrange("r (c k) -> (r c) k", k=k)
                dv = dst.rearrange("r (c k) -> (r c) k", k=k)
                n = rows * (cols // k)
                for a in range(0, n, 512):
                    P.add(POOL, lambda e, o=dv[a:a + 512, :], i=sv[a:a + 512, :]: e.dma_start(out=o, in_=i),
                          (), writes(a), deps=deps, dma=True)
            if "C" in phases:
                precast(WINb, w_in, D, 7680, lambda a: [WcB])
                precast(WAPb, w_ap, 1024, D, lambda a: [WcB])
                precast(WGPb, w_gp, 1024, D, lambda a: [WcB])
                precast(WOUTb, w_out, D, D, lambda a: [WcB])
            if "E" in phases:
                precast(WPQb, w_pq, D, D, lambda a: [WcB])
                precast(UB16, peer_u, NEXP, D, lambda a: [UcB[a // 512]])
                precast(VB16, peer_v, NEXP, D, lambda a: [VcB[a // 512]])

        w_in_v = w_in.rearrange("(dt p) n -> p dt n", p=128)

        if "A" in phases:
            with ExitStack() as es:
                def sb(name, shape, dt):
                    return es.enter_context(nc.sbuf_tensor("A_" + name, list(shape), dt))

                def ps(name, shape, dt):
                    return es.enter_context(nc.psum_tensor("A_" + name, list(shape), dt))

                R = {
                    "xt": Ring(sb, "xt", [128, D], F32, 3),
                    "st": Ring(sb, "st", [128, 4], F32, 4),
                    "xs": Ring(sb, "xs", [128, D], BF16, 2),
                    "pT": Ring(ps, "pT", [128, 16, 128], BF16, 1),
                    "sq": sb("sq", [128, 1024], F32), "kn": sb("kn", [128, 1024], F32),
                    "t1": sb("t1", [128, 512], F32), "t2": sb("t2", [128, 512], F32),
                    "t3": sb("t3", [128, 512], F32), "t4": sb("t4", [128, 512], F32), "ropeB2": Buf(),
                    "sm": sb("sm", [128, 48], F32), "ropeB": Buf(),
                }
                xsT_r = Ring(sb, "xsT", [128, 16, 128], BF16, 2)
                xsT_hiB = [Buf(), Buf()]
                cs_r = Ring(sb, "cst", [128, 128], F32, 4)
                wkv = sb("wkv", [128, 16, 512], BF16)
                wq = sb("wq", [128, 16, 1024], BF16)
                gqk = sb("gqk", [128, 256], F32)
                WB = Buf("wA")
                wload(wkv[:], w_in_v[:, :, C_K:C_K + 512], [WB])
                wload(wq[:], w_in_v[:, :, C_Q:C_Q + 1024], [WB], nsplit=8)
                if "B" not in phases:
                    emit_precasts([])
                dma(gqk[:, 0:128], q_norm[0, :].partition_broadcast(128), writes=[CB])
                dma(gqk[:, 128:256], k_norm[0, :].partition_broadcast(128), writes=[CB])
                pbig = Ring(ps, "pbig", [128, 1024], F32, 2)
                pKT = Ring(ps, "pKT", [128, 8, 128], BF16, 2)
                kr_r = Ring(sb, "kr", [128, 8, 128], BF16, 2)
                kvs_r = Ring(sb, "kvs", [128, 1024], F32, 2)
                vb_r = Ring(sb, "vb", [128, 2, 128], BF16, 3)
                kT_r = Ring(sb, "kTs", [128, 2, 128], BF16, 3)

                def a_stage0(xsrc, cssrc, i):
                    cst, cstB = cs_r.next()
                    dma(cst[:], cssrc[i * 128:(i + 1) * 128, :], writes=[cstB])
                    xt, xtB = R["xt"].next()
                    dma(xt[:], xsrc[i * 128:(i + 1) * 128, :], writes=[xtB])
                    return (cst, cstB, xt, xtB)

                def a1_stage1(i, ld):
                    hiB = xsT_hiB[xsT_r.i % 2]
                    xsT, xsTB = xsT_r.next()
                    cst, cstB, xt, xtB = ld
                    st, stB = norm_T_sb(xt[:], xtB, gmixT, xsT[:], xsTB, R, defer=True, dstB2=hiB)
                    pk, pkB = pbig.next()
                    for dt in range(16):
                        mm(pk[:, 0:512], xsT[:, dt, :], wkv[:, dt, :], dt == 0, dt == 15,
                           reads=[xsTB if dt < 8 else hiB, WB], writes=[pkB])
                    return (i, pk, pkB, cst, cstB, st, stB)

                def a1_stage2(stt):
                    i, pk, pkB, cst, cstB, st, stB = stt
                    kvs, kvsB = kvs_r.next()
                    act(kvs[:, 0:512], pk[:, 0:512], AF.Copy, reads=[pkB, stB], writes=[kvsB], scale=st[:, 2:3])
                    kr, krB = kr_r.next()
                    qk_norm_rope(kvs[:, 0:256], kvsB, 2, cap(gqk[:, 128:256], [(0, 2), (1, 128)]), cst, cstB,
                                 kr[:, 0:2, :], krB, R)
                    vb, vbB = vb_r.next()
                    cp(vb[:].rearrange("p h d -> p (h d)"), kvs[:, 256:512], reads=[kvsB], writes=[vbB], eng=POOL)
                    pt_, ptB = pKT.next()
                    for h in range(2):
                        tr(pt_[:, h, :], kr[:, h, :], ident_b[:], reads=[krB, CB], writes=[ptB])
                    def late():
                        kTs, kTB = kT_r.next()
                        cp(kTs[:], pt_[:, 0:2, :], reads=[ptB], writes=[kTB])
                        dma(KT[:, :, i * 128:(i + 1) * 128].rearrange("h d t -> d h t"), kTs[:], reads=[kTB],
                            writes=[KTB[0][i], KTB[1][i]])
                        dma(VD[:, i * 128:(i + 1) * 128, :].rearrange("h t d -> t h d"), vb[:], reads=[vbB],
                            writes=[VDB[0][i], VDB[1][i]])
                    return late

                def a2_stage1(i, ld):
                    hiB = xsT_hiB[xsT_r.i % 2]
                    xsT, xsTB = xsT_r.next()
                    cst, cstB, xt, xtB = ld
                    st, stB = norm_T_sb(xt[:], xtB, gmixT, xsT[:], xsTB, R, defer=True, dstB2=hiB)
                    pq, pqB = pbig.next()
                    for half in range(2):
                        for dt in range(16):
                            mm(pq[:, half * 512:(half + 1) * 512], xsT[:, dt, :], wq[:, dt, half * 512:(half + 1) * 512],
                               dt == 0, dt == 15, reads=[xsTB if dt < 8 else hiB, WB], writes=[pqB])
                    return (i, pq, pqB, cst, cstB, st, stB)

                def a2_stage2(stt):
                    i, pq, pqB, cst, cstB, st, stB = stt
                    kvs, kvsB = kvs_r.next()
                    act(kvs[:], pq[:], AF.Copy, reads=[pqB, stB], writes=[kvsB], scale=st[:, 2:3])
                    kr, krB = kr_r.next()
                    qk_norm_rope(kvs[:], kvsB, 8, cap(gqk[:, 0:128], [(0, 8), (1, 128)]), cst, cstB, kr[:], krB, R)
                    pt_, ptB = pKT.next()
                    for h in range(8):
                        tr(pt_[:, h, :], kr[:, h, :], ident_b[:], reads=[krB, CB], writes=[ptB])
                    def late():
                        cp(QA[:, :, i * 128:(i + 1) * 128], pt_[:], reads=[ptB], writes=[QAB[0][i], QAB[1][i]])
                    return late

                work = [(a1_stage1, a1_stage2, i, x_all, cs_all) for i in range(NT_ALL)] + \
                       [(a2_stage1, a2_stage2, i, x_own, cs_own) for i in range(NT_OWN)]
                NW = len(work)
                lds = {}
                for w in range(min(2, NW)):
                    lds[w] = a_stage0(work[w][3], work[w][4], work[w][2])
                cur = work[0][0](work[0][2], lds.pop(0))
                late_prev = None
                for w in range(NW):
                    if w + 2 < NW:
                        lds[w + 2] = a_stage0(work[w + 2][3], work[w + 2][4], work[w + 2][2])
                    nxt = work[w + 1][0](work[w + 1][2], lds.pop(w + 1)) if w + 1 < NW else None
                    if late_prev is not None:
                        late_prev()
                    late_prev = work[w][1](cur)
                    cur = nxt
                late_prev()
                if dbg:
                    final_ops.append(dma(QAd, QA[:], reads=[b for r in QAB for b in r]))
            P.barrier()

        if "B" in phases:
            with ExitStack() as es:
                def sb(name, shape, dt):
                    return es.enter_context(nc.sbuf_tensor("B_" + name, list(shape), dt))

                def ps(name, shape, dt):
                    return es.enter_context(nc.psum_tensor("B_" + name, list(shape), dt))

                KTs2 = [sb("KTs%d" % h, [128, S], BF16) for h in range(2)]
                V12 = [sb("V1%d" % h, [128, NT_ALL, 130], BF16) for h in range(2)]
                NSEG = 8 if NT_ALL >= 8 else (4 if NT_ALL >= 4 else 1)
                SEG = NT_ALL // NSEG
                KB2 = [[Buf() for _ in range(NSEG)] for h in range(2)]
                VB2 = [[Buf() for _ in range(NSEG)] for h in range(2)]
                pS = Ring(ps, "pS", [128, 512], F32, 3)
                pO = [ps("pO%d" % g, [128, 512], F32) for g in range(4)]
                pOB = [Buf() for _ in range(4)]
                pOT = Ring(ps, "pOT", [128, 4, 128], BF16, 1)
                pt_r = Ring(sb, "pt", [128, 512], BF16, 4)
                ob_r = Ring(sb, "ob", [128, 128], BF16, 4)
                rc_r = Ring(sb, "rc", [128, 1], F32, 4)
                P.add(POOL, lambda e: e.memset(V12[0][:], 1.0), writes=VB2[0])
                P.add(POOL, lambda e: e.memset(V12[1][:], 1.0), writes=VB2[1])
                kv_loads = []
                for kvh in range(2):
                    for sg in range(NSEG):
                        a, b = sg * SEG, (sg + 1) * SEG
                        kv_loads.append(dma(KTs2[kvh][:, a * 128:b * 128], KT[kvh, :, a * 128:b * 128],
                                            reads=KTB[kvh][a:b], writes=[KB2[kvh][sg]]))
                        kv_loads.append(dma(V12[kvh][:, a:b, 0:128],
                                            VD[kvh, a * 128:b * 128, :].rearrange("(n p) d -> p n d", p=128),
                                            reads=VDB[kvh][a:b], writes=[VB2[kvh][sg]]))
                emit_precasts(kv_loads)
                scale = 1.0 / float(np.sqrt(128.0))
                for kvh in range(2):
                    KTs, V1, KB, VB = KTs2[kvh], V12[kvh], KB2[kvh], VB2[kvh]
                    steps = [(qt, st) for qt in range(NT_OWN) for st in range(NT_ALL)]
                    LA = 2
                    pend = []

                    def emit_qk(idx):
                        qt, st = steps[idx]
                        qsl_ = QA[:, 4 * kvh:4 * kvh + 4, qt * 128:(qt + 1) * 128]
                        p_s, p_sB = pS.next()
                        mm(p_s[:], KTs[:, st * 128:(st + 1) * 128], qsl_, True, True,
                           reads=[KB[st // SEG], QAB[kvh][qt]], writes=[p_sB])
                        pend.append((p_s, p_sB))

                    for idx in range(min(LA, len(steps))):
                        emit_qk(idx)
                    for idx, (qt, st) in enumerate(steps):
                        if idx + LA < len(steps):
                            emit_qk(idx + LA)
                        qB = QAB[kvh][qt]
                        qsl = QA[:, 4 * kvh:4 * kvh + 4, qt * 128:(qt + 1) * 128]
                        sg = st // SEG
                        p_s, p_sB = pend.pop(0)
                        pt, ptB = pt_r.next()
                        act(pt[:], p_s[:], AF.Exp, reads=[p_sB], writes=[ptB], scale=scale)
                        for g in range(4):
                            mm(pO[g][:, 0:129], pt[:, g * 128:(g + 1) * 128], V1[:, st, 0:129], st == 0,
                               st == NT_ALL - 1, reads=[ptB, VB[sg]], writes=[pOB[g]])
                        if st == NT_ALL - 1:
                            po_t, po_tB = pOT.next()
                            for g in range(4):
                                rc, rcB = rc_r.next()
                                ob, obB = ob_r.next()
                                rcp(rc[:], pO[g][:, 128:129], reads=[pOB[g]], writes=[rcB])
                                ts(ob[:], pO[g][:, 0:128], rc[:, 0:1], None, ALU.mult, reads=[pOB[g], rcB], writes=[obB])
                                tr(po_t[:, g, :], ob[:], ident_b[:], reads=[obB, CB], writes=[po_tB])
                            act(qsl, po_t[:], AF.Copy, reads=[po_tB], writes=[qB])
                if dbg:
                    final_ops.append(dma(ATd, QA[:], reads=[b for r in QAB for b in r]))
            P.barrier()

        if "C" in phases:
            with ExitStack() as es:
                def sb(name, shape, dt):
                    return es.enter_context(nc.sbuf_tensor("C_" + name, list(shape), dt))

                def ps(name, shape, dt):
                    return es.enter_context(nc.psum_tensor("C_" + name, list(shape), dt))

                R = {
                    "xt": Ring(sb, "xt", [128, D], F32, 2),
                    "st": Ring(sb, "st", [128, 4], F32, 2),
                    "xs": Ring(sb, "xs", [128, D], BF16, 2),
                    "pT": Ring(ps, "pT", [128, 16, 128], BF16, 1),
                    "junk": sb("junk", [128, 1024], BF16), "junkB": Buf(),
                }
                wsT = sb("wsT", [128, 8, 128], BF16)
                bs_bc = sb("bs_bc", [128, 8, 128], F32)
                gsg = sb("gsg", [128, 1024], F32)
                bgT = sb("bgT", [128, 32], F32)
                with ExitStack() as es2:
                    load_colvec(bgT, b_gate, 32, es2)
                    wtmp = es2.enter_context(nc.sbuf_tensor("wtmp", [128, 8, 128], F32))
                    pw = es2.enter_context(nc.psum_tensor("pw", [128, 8, 128], F32))
                    b = Buf()
                    dma(wtmp[:], w_sgu.rearrange("g p q -> p g q"), writes=[b])
                    for g in range(8):
                        tr(pw[:, g, :], wtmp[:, g, :], ident_f[:], reads=[b, CB], writes=[b])
                    cp(wsT[:], pw[:], reads=[b], writes=[CB])
                    dma(bs_bc[:].rearrange("p g q -> p (g q)"), b_sgu[0, :].partition_broadcast(128), writes=[CB])
                    dma(gsg[:], sgu_norm[0, :].partition_broadcast(128), writes=[CB])
                    P.barrier()
                xsTc = sb("xsTc", [128, 16, TC], BF16)
                xsTcB = [Buf() for _ in range(TC // 128)]
                sguT = sb("sguT", [128, 8, TC], BF16)
                sguB = Buf()
                mT = sb("mT", [128, 16, TC], BF16)
                mTB = [Buf() for _ in range(16)]
                WBK = 256
                NJ = WBK // 128
                wblk = Ring(sb, "wblk", [128, 16, WBK], BF16, 4)
                wblk2 = Ring(sb, "wblk2", [128, 8, WBK], BF16, 4)
                gv = sb("gv", [128, 1024], F32)
                gvB = Buf()
                vn_r = Ring(sb, "vn", [128, 1024], BF16, 2)
                tmpm = sb("tmpm", [128, 8, 128], F32)
                tmpmB = Buf()
                sig_r = Ring(sb, "sig", [128, 512], F32, 4)
                m1_r = Ring(sb, "m1", [128, 512], F32, 4)
                xr_r = Ring(sb, "xr", [128, WBK], F32, 4)
                h2_r = Ring(sb, "h2t", [128, WBK], F32, 3)
                pbk = [ps("pbk%d" % k, [128, 512], F32) for k in range(6)]
                pbB = [Buf() for _ in range(6)]
                pbi = [0]

                def nextbank():
                    k = pbi[0] % 6
                    pbi[0] += 1
                    return pbk[k], pbB[k]

                win_v = WINb.rearrange("(dt p) n -> p dt n", p=128)
                wap_v = WAPb.rearrange("(kt p) n -> p kt n", p=128)
                wgp_v = WGPb.rearrange("(kt p) n -> p kt n", p=128)
                wout_v = WOUTb.rearrange("(kt p) n -> p kt n", p=128)

                def wl(ring, src):
                    wb, wbB = ring.next()
                    dma(wb[:], src, reads=[WcB], writes=[wbB], q=POOL)
                    return wb, wbB

                NTC = TC // 128
                for c in range(NCH):
                    for tl in range(NTC):
                        row = c * TC + tl * 128
                        norm_T(x_own[row:row + 128, :], gmixT, xsTc[:, :, tl * 128:(tl + 1) * 128], xsTcB[tl], R)
                    for blk in range(1024 // WBK):
                        wb, wbB = wl(wblk, win_v[:, :, C_SU + blk * WBK:C_SU + (blk + 1) * WBK])
                        for j in range(NJ):
                            f = blk * NJ + j
                            pb, pbB_ = nextbank()
                            for dt in range(16):
                                mm(pb[:], wb[:, dt, j * 128:(j + 1) * 128], xsTc[:, dt, :], dt == 0, dt == 15,
                                   reads=[wbB] + xsTcB, writes=[pbB_])
                            act(sguT[:, f, :], pb[:], AF.Gelu_apprx_tanh, reads=[pbB_], writes=[sguB])
                    wsv = [wl(wblk, win_v[:, :, C_SV + blk * WBK:C_SV + (blk + 1) * WBK]) for blk in range(1024 // WBK)]
                    for tl in range(NTC):
                        banks = [nextbank(), nextbank()]
                        for blk in range(1024 // WBK):
                            wb, wbB = wsv[blk]
                            pb, pbB_ = banks[(blk * WBK) // 512]
                            co = (blk * WBK) % 512
                            for dt in range(16):
                                mm(pb[:, co:co + WBK], xsTc[:, dt, tl * 128:(tl + 1) * 128], wb[:, dt, :], dt == 0, dt == 15,
                                   reads=[wbB, xsTcB[tl]], writes=[pbB_])
                        st, stB = R["st"].next()
                        for blk in range(2):
                            act(gv[:, blk * 512:(blk + 1) * 512], banks[blk][0][:], AF.Gelu_apprx_tanh,
                                reads=[banks[blk][1]], writes=[gvB])
                        act(R["junk"][:], gv[:], AF.Square, reads=[gvB], writes=[R["junkB"], stB],
                            accum_out=st[:, 0:1])
                        act(st[:, 1:2], st[:, 0:1], AF.Sqrt, reads=[stB], writes=[stB], scale=1.0 / 1024, bias=EPS)
                        rcp(st[:, 2:3], st[:, 1:2], reads=[stB], writes=[stB])
                        vn, vnB = vn_r.next()
                        P.add(DVE, lambda e, vn=vn, st=st: e.scalar_tensor_tensor(
                            out=vn[:], in0=gv[:], scalar=st[:, 2:3], in1=gsg[:], op0=ALU.mult, op1=ALU.mult),
                            reads=[gvB, stB, CB], writes=[vnB])
                        pm0, pm0B = nextbank()
                        pm1, pm1B = nextbank()
                        for g in range(8):
                            pm, pmB = (pm0, pm0B) if g < 4 else (pm1, pm1B)
                            mm(pm[:, (g % 4) * 128:(g % 4 + 1) * 128], vn[:, g * 128:(g + 1) * 128], wsT[:, g, :],
                               True, True, reads=[vnB, CB], writes=[pmB])
                        for hh, (pm, pmB) in enumerate(((pm0, pm0B), (pm1, pm1B))):
                            tt(tmpm[:, hh * 4:(hh + 1) * 4, :], pm[:].rearrange("p (g q) -> p g q", q=128),
                               bs_bc[:, hh * 4:(hh + 1) * 4, :], ALU.add, reads=[pmB, CB], writes=[tmpmB])
                        tt(sguT[:, :, tl * 128:(tl + 1) * 128], sguT[:, :, tl * 128:(tl + 1) * 128], tmpm[:], ALU.mult,
                           reads=[tmpmB, sguB], writes=[sguB])
                    if dbg:
                        final_ops.append(dma(SGd[:, :, c * TC:(c + 1) * TC], sguT[:], reads=[sguB]))
                    for blk in range(D // WBK):
                        wga, wgaB = wl(wblk, win_v[:, :, C_GA + blk * WBK:C_GA + (blk + 1) * WBK])
                        wgg, wggB = wl(wblk, win_v[:, :, C_GG + blk * WBK:C_GG + (blk + 1) * WBK])
                        wpa, wpaB = wl(wblk2, wap_v[:, :, blk * WBK:(blk + 1) * WBK])
                        wpg, wpgB = wl(wblk2, wgp_v[:, :, blk * WBK:(blk + 1) * WBK])
                        for j in range(NJ):
                            dm = blk * NJ + j
                            js = slice(j * 128, (j + 1) * 128)
                            sigs = []
                            for (wg, wgB, col) in ((wga, wgaB, dm), (wgg, wggB, 16 + dm)):
                                pb, pbB_ = nextbank()
                                for dt in range(16):
                                    mm(pb[:], wg[:, dt, js], xsTc[:, dt, :], dt == 0, dt == 15,
                                       reads=[wgB] + xsTcB, writes=[pbB_])
                                sg_, sgB = sig_r.next()
                                act(sg_[:], pb[:], AF.Sigmoid, reads=[pbB_, CB], writes=[sgB], bias=bgT[:, col:col + 1])
                                sigs.append((sg_, sgB))
                            ms = []
                            for k, (wp, wpB) in enumerate(((wpa, wpaB), (wpg, wpgB))):
                                pb, pbB_ = nextbank()
                                for kt in range(8):
                                    if k == 0:
                                        rhs = QA[:, kt, c * TC:(c + 1) * TC]
                                        rds = [wpB] + [QAB[kt // 4][c * NTC + u] for u in range(NTC)]
                                    else:
                                        rhs = sguT[:, kt, :]
                                        rds = [wpB, sguB]
                                    mm(pb[:], wp[:, kt, js], rhs, kt == 0, kt == 7, reads=rds, writes=[pbB_])
                                m1, m1B = m1_r.next()
                                tt(m1[:], sigs[k][0][:], pb[:], ALU.mult, reads=[sigs[k][1], pbB_], writes=[m1B])
                                ms.append((m1, m1B))
                            tt(mT[:, dm, :], ms[0][0][:], ms[1][0][:], ALU.add, reads=[ms[0][1], ms[1][1]],
                               writes=[mTB[dm]])
                    if dbg:
                        final_ops.append(dma(MTd[:, :, c * TC:(c + 1) * TC], mT[:], reads=mTB))
                    steps4 = [(cb, tl) for cb in range(D // WBK) for tl in range(NTC)]
                    xrq = {}

                    def xr_load(k):
                        cb, tl = steps4[k]
                        row = c * TC + tl * 128
                        xr, xrB = xr_r.next()
                        dma(xr[:], x_own[row:row + 128, cb * WBK:(cb + 1) * WBK], writes=[xrB])
                        xrq[k] = (xr, xrB)

                    for k in range(min(2, len(steps4))):
                        xr_load(k)
                    wo = woB = None
                    for k, (cb, tl) in enumerate(steps4):
                        if tl == 0:
                            wo, woB = wl(wblk, wout_v[:, :, cb * WBK:(cb + 1) * WBK])
                        if k + 2 < len(steps4):
                            xr_load(k + 2)
                        row = c * TC + tl * 128
                        xr, xrB = xrq.pop(k)
                        pb, pbB_ = nextbank()
                        for dm in range(16):
                            mm(pb[:, 0:WBK], mT[:, dm, tl * 128:(tl + 1) * 128], wo[:, dm, :], dm == 0, dm == 15,
                               reads=[woB, mTB[dm]], writes=[pbB_])
                        h2t, h2B_ = h2_r.next()
                        tt(h2t[:], xr[:], pb[:, 0:WBK], ALU.add, reads=[xrB, pbB_], writes=[h2B_])
                        o = dma(H2[row:row + 128, cb * WBK:(cb + 1) * WBK], h2t[:], reads=[h2B_],
                                writes=[H2B[c * NTC + tl]])
                        if dbg:
                            final_ops.append(o)
            P.barrier()

        esQA.close()
        if "E" in phases:
            with ExitStack() as es:
                def sb(name, shape, dt):
                    return es.enter_context(nc.sbuf_tensor("E_" + name, list(shape), dt))

                def ps(name, shape, dt):
                    return es.enter_context(nc.psum_tensor("E_" + name, list(shape), dt))

                NTC = TC // 128
                AG = 64
                NAG = 128 // AG
                KxT = sb("KxT", [128, 16, 128], F32)
                with ExitStack() as es2:
                    ktmp = es2.enter_context(nc.sbuf_tensor("ktmp", [128, 16, 128], F32))
                    pk_ = es2.enter_context(nc.psum_tensor("pk_", [128, 16, 128], F32))
                    b = Buf()
                    kview = ktmp[:].rearrange("p (h two) d -> p h two d", two=2)
                    dma(kview[:, :, 0, :], pk1.rearrange("h n d -> n h d"), writes=[b])
                    dma(kview[:, :, 1, :], pk2.rearrange("h n d -> n h d"), writes=[b])
                    for j in range(16):
                        tr(pk_[:, j, :], ktmp[:, j, :], ident_f[:], reads=[b, CB], writes=[b])
                    cp(KxT[:], pk_[:], reads=[b], writes=[CB])
                    P.barrier()
                R = {
                    "st": Ring(sb, "st", [128, 4], F32, 2),
                    "xs": Ring(sb, "xs", [128, D], BF16, 1),
                    "pT": Ring(ps, "pT", [128, 16, 128], BF16, 2),
                }
                Gt = sb("Gt", [128, AG * TC], BF16)
                G3 = Gt[:].rearrange("p (a t) -> p a t", t=TC)
                qpT = Gt[:].bitcast(F32).rearrange("p (j t) -> p j t", t=TC)
                GB = Buf("G")
                oacc = sb("oacc", [128, NTC, D], F32)
                oaccB = [Buf() for _ in range(NTC)]
                xn2T = sb("xn2T", [128, 16, TC], BF16)
                xn2B = [Buf() for _ in range(NTC)]
                rT = sb("rT", [128, 3, TC], F32)
                rTB = Buf()
                wpq_r = Ring(sb, "wpq", [128, 16, 128], BF16, 2)
                ub_r = Ring(sb, "ub", [128, D], BF16, 3)
                uT_r = Ring(sb, "uT", [128, 16, 128], BF16, 2)
                vb_r = Ring(sb, "vblk", [128, 512], BF16, 6)
                hb_r = Ring(sb, "hb", [128, TC], BF16, 2)
                ohb_r = Ring(sb, "ohb", [128, 8, 128], BF16, 2)
                oha_r = Ring(sb, "oha", [128, 8, AG], BF16, 3)
                vv = sb("vv", [128, 16, 16], F32)
                vi = sb("vi", [128, 16, 16], U32)
                vif = sb("vif", [128, 16, 16], F32)
                wk = sb("wk", [128, 16, 128], F32)
                cand = sb("cand", [128, 8, 256], F32)
                cwk = sb("cwk", [128, 8, 256], F32)
                cv = sb("cv", [128, 8, 16], F32)
                ci = sb("ci", [128, 8, 16], U32)
                cih = sb("cih", [128, 8, 16], U32)
                cil = sb("cil", [128, 8, 16], U32)
                cihf = sb("cihf", [128, 8, 16], F32)
                cilf = sb("cilf", [128, 8, 16], F32)
                abg = sb("abg", [128, 3, 128], F32)
                zz = sb("zz", [128, 16], F32)
                RB = Buf("route")
                L1B = [Buf() for _ in range(16)]
                L2B = [Buf() for _ in range(8)]
                w_pq_v = WPQb.rearrange("(dt p) n -> p dt n", p=128)
                pb4 = [ps("pe%d" % k, [128, 512], F32) for k in range(4)]
                pb4B = [Buf() for _ in range(4)]
                pei = [0]

                def nextbank4(lo=0, hi=4):
                    k = lo + pei[0] % (hi - lo)
                    pei[0] += 1
                    return pb4[k], pb4B[k]

                for c in range(NCH):
                    for tl in range(NTC):
                        row = c * TC + tl * 128
                        dma(oacc[:, tl, :], H2[row:row + 128, :], reads=[H2B[c * NTC + tl]], writes=[oaccB[tl]])

                    for tl in range(NTC):
                        norm_T_sb(oacc[:, tl, :], oaccB[tl], gffnT, xn2T[:, :, tl * 128:(tl + 1) * 128], xn2B[tl], R)
                    for j in range(16):
                        wb, wbB = wpq_r.next()
                        dma(wb[:], w_pq_v[:, :, j * 128:(j + 1) * 128], reads=[WcB], writes=[wbB], q=POOL)
                        pb, pbB_ = nextbank4()
                        for dt in range(16):
                            mm(pb[:], wb[:, dt, :], xn2T[:, dt, :], dt == 0, dt == 15, reads=[wbB] + xn2B, writes=[pbB_])
                        act(qpT[:, j, :], pb[:], AF.Copy, reads=[pbB_], writes=[GB])
                    for tl in range(NTC):
                        tsl = slice(tl * 128, (tl + 1) * 128)
                        sbk = []
                        for q4 in range(4):
                            pb, pbB_ = nextbank4(0, 4)
                            sbk.append((pb, pbB_))
                            for jj in range(4):
                                j = q4 * 4 + jj
                                mm(pb[:, jj * 128:(jj + 1) * 128], qpT[:, j, tsl], KxT[:, j, :], True, True,
                                   reads=[GB, CB], writes=[pbB_])

                        def sc(j):
                            return sbk[j // 4][0][:, (j % 4) * 128:(j % 4 + 1) * 128], sbk[j // 4][1]
                        for j in range(16):
                            s_, sB = sc(j)
                            P.add(DVE, lambda e, j=j, s_=s_: e.max(out=vv[:, j, 0:8], in_=s_), reads=[sB], writes=[L1B[j]])
                        for j in range(16):
                            s_, sB = sc(j)
                            P.add(DVE, lambda e, j=j, s_=s_: e.max_index(out=vi[:, j, 0:8], in_max=vv[:, j, 0:8], in_values=s_),
                                  reads=[sB, L1B[j]], writes=[L1B[j]])
                        for j in range(16):
                            s_, sB = sc(j)
                            P.add(DVE, lambda e, j=j, s_=s_: e.match_replace(out=wk[:, j, :], in_to_replace=vv[:, j, 0:8],
                                                                            in_values=s_, imm_value=-1e30),
                                  reads=[sB, L1B[j]], writes=[L1B[j]])
                        for j in range(16):
                            P.add(DVE, lambda e, j=j: e.max(out=vv[:, j, 8:16], in_=wk[:, j, :]), reads=[L1B[j]], writes=[L1B[j]])
                        for j in range(16):
                            P.add(DVE, lambda e, j=j: e.max_index(out=vi[:, j, 8:16], in_max=vv[:, j, 8:16], in_values=wk[:, j, :]),
                                  reads=[L1B[j]], writes=[L1B[j]])
                        cp(vif[:], vi[:], reads=L1B, writes=[RB])
                        tt(cand[:].rearrange("p h (i j) -> p h i j", j=16),
                           cap(vv[:, 0, :], [(32, 8), (1, 16), (0, 16)]),
                           cap(vv[:, 1, :], [(32, 8), (0, 16), (1, 16)]), ALU.add, reads=L1B, writes=[RB])
                        for h in range(8):
                            P.add(DVE, lambda e, h=h: e.max(out=cv[:, h, 0:8], in_=cand[:, h, :]), reads=[RB], writes=[L2B[h]])
                        for h in range(8):
                            P.add(DVE, lambda e, h=h: e.max_index(out=ci[:, h, 0:8], in_max=cv[:, h, 0:8], in_values=cand[:, h, :]),
                                  reads=[RB, L2B[h]], writes=[L2B[h]])
                        for h in range(8):
                            P.add(DVE, lambda e, h=h: e.match_replace(out=cwk[:, h, :], in_to_replace=cv[:, h, 0:8],
                                                                      in_values=cand[:, h, :], imm_value=-1e30),
                                  reads=[RB, L2B[h]], writes=[L2B[h]])
                        for h in range(8):
                            P.add(DVE, lambda e, h=h: e.max(out=cv[:, h, 8:16], in_=cwk[:, h, :]), reads=[L2B[h]], writes=[L2B[h]])
                        for h in range(8):
                            P.add(DVE, lambda e, h=h: e.max_index(out=ci[:, h, 8:16], in_max=cv[:, h, 8:16], in_values=cwk[:, h, :]),
                                  reads=[L2B[h]], writes=[L2B[h]])
                        P.add(DVE, lambda e: e.tensor_single_scalar(out=cih[:], in_=ci[:], scalar=4, op=ALU.logical_shift_right),
                              reads=L2B, writes=[RB])
                        P.add(DVE, lambda e: e.tensor_single_scalar(out=cil[:], in_=ci[:], scalar=15, op=ALU.bitwise_and),
                              reads=L2B, writes=[RB])
                        cp(cihf[:], cih[:], reads=[RB], writes=[RB])
                        cp(cilf[:], cil[:], reads=[RB], writes=[RB])
                        oh = wk[:].rearrange("p j n -> p (j n)")
                        oh4 = oh.rearrange("p (h k i) -> p h k i", k=16, i=16)
                        for which, (sel, tab) in enumerate(((cihf, 0), (cilf, 1))):
                            tt(oh4, cap(iota_f[:, 0:16], [(0, 8), (0, 16), (1, 16)]),
                               cap(sel[:, 0, :], [(16, 8), (1, 16), (0, 16)]), ALU.is_equal, reads=[RB, CB], writes=[RB] + L1B)
                            tt(oh4, oh4, cap(vif[:, tab, :], [(32, 8), (0, 16), (1, 16)]), ALU.mult, reads=[RB], writes=[RB] + L1B)
                            red(abg[:, which, :], oh.rearrange("p (hk i) -> p hk i", i=16), reads=[RB] + L1B, writes=[RB])
                        tt(cwk[:, :, 0:16], cv[:], cap(cv[:, 0, 0:1], [(16, 8), (0, 16)]), ALU.subtract, reads=L2B, writes=[RB] + L2B)
                        act(cwk[:, :, 0:16], cwk[:, :, 0:16], AF.Exp, reads=[RB], writes=[RB] + L2B)
                        red(zz[:, 0:8], cwk[:, :, 0:16], reads=[RB], writes=[RB])
                        rcp(zz[:, 8:16], zz[:, 0:8], reads=[RB], writes=[RB])
                        tt(abg[:, 2, :].rearrange("p (h k) -> p h k", k=16), cwk[:, :, 0:16],
                           cap(zz[:, 8:16], [(1, 8), (0, 16)]), ALU.mult, reads=[RB], writes=[RB])
                        pTt, pbB_ = R["pT"].next()
                        pb = pTt[:].rearrange("p a b -> p (a b)").bitcast(F32)
                        for w3 in range(3):
                            tr(pb[:, w3 * 128:(w3 + 1) * 128], abg[:, w3, :], ident_f[:], reads=[RB, CB], writes=[pbB_])
                        cp(rT[:, :, tsl], pb[:, 0:384].rearrange("p (w t) -> p w t", t=128), reads=[pbB_], writes=[rTB])
                    if dbg:
                        final_ops.append(dma(RTd[:, :, c * TC:(c + 1) * TC].rearrange("w p t -> p w t"), rT[:], reads=[rTB]))
                    P.barrier()
                    for ag in range(NAG):
                        ioT, ioB = R["pT"].next()
                        ioT2, ioB2 = R["pT"].next()
                        io_b = ioT[:].rearrange("p a b -> p (a b)").bitcast(F32).rearrange("p (t b) -> p t b", b=128)
                        io_a = ioT2[:].rearrange("p a b -> p (a b)").bitcast(F32)[:, 0:8 * AG].rearrange("p (t a) -> p t a", a=AG)
                        cp(io_b, cap(iota_f[:, 0:128], [(0, 8), (1, 128)]), reads=[CB], writes=[ioB])
                        cp(io_a, cap(iota_f[:, ag * AG:(ag + 1) * AG], [(0, 8), (1, AG)]), reads=[CB], writes=[ioB2])
                        for t0 in range(0, TC, 8):
                            ohb, ohbB = ohb_r.next()
                            oha, ohaB = oha_r.next()
                            pb, pbB_ = nextbank4(0, 4)
                            tt(ohb[:], io_b, cap(rT[:, 1, t0:t0 + 8], [(1, 8), (0, 128)]), ALU.is_equal,
                               reads=[rTB, ioB], writes=[ohbB])
                            tt(oha[:], io_a, cap(rT[:, 0, t0:t0 + 8], [(1, 8), (0, AG)]), ALU.is_equal,
                               reads=[rTB, ioB2], writes=[ohaB])
                            tt(oha[:], oha[:], cap(rT[:, 2, t0:t0 + 8], [(1, 8), (0, AG)]), ALU.mult,
                               reads=[rTB], writes=[ohaB], eng=POOL)
                            for u in range(8):
                                mm(pb[:, u * AG:(u + 1) * AG], ohb[:, u, :], oha[:, u, :], True, True,
                                   reads=[ohbB, ohaB], writes=[pbB_])
                            act(G3[:, :, t0:t0 + 8], pb[:, 0:8 * AG].rearrange("p (t a) -> p a t", a=AG), AF.Copy,
                                reads=[pbB_], writes=[GB])
                        def e2_front(al):
                            a = ag * AG + al
                            ub, ubB = ub_r.next()
                            dma(ub[:], UB16[a * 128:(a + 1) * 128, :], reads=[UcB[a // 4]], writes=[ubB], q=POOL)
                            pT, pTB = R["pT"].next()
                            for dt in range(16):
                                tr(pT[:, dt, :], ub[:, dt * 128:(dt + 1) * 128], ident_b[:], reads=[ubB, CB], writes=[pTB])
                            uT, uTB = uT_r.next()
                            cp(uT[:], pT[:], reads=[pTB], writes=[uTB])
                            return uT, uTB

                        fr = e2_front(0)
                        for al in range(AG):
                            nfr = e2_front(al + 1) if al + 1 < AG else None
                            uT, uTB = fr
                            pb, pbB_ = nextbank4(0, 2)
                            for dt in range(16):
                                mm(pb[:], uT[:, dt, :], xn2T[:, dt, :], dt == 0, dt == 15, reads=[uTB] + xn2B, writes=[pbB_])
                            hb, hbB = hb_r.next()
                            act(hb[:], pb[:], AF.Gelu_apprx_tanh, reads=[pbB_], writes=[hbB])
                            tt(G3[:, al, :], hb[:], G3[:, al, :], ALU.mult, reads=[hbB, GB], writes=[GB])
                            fr = nfr
                        for db in range(4):
                            banks = [(pb4[tl], pb4B[tl]) for tl in range(NTC)]
                            for al in range(AG):
                                a = ag * AG + al
                                vb, vbB = vb_r.next()
                                dma(vb[:], VB16[a * 128:(a + 1) * 128, db * 512:(db + 1) * 512], reads=[VcB[a // 4]], writes=[vbB])
                                for tl in range(NTC):
                                    mm(banks[tl][0][:], G3[:, al, tl * 128:(tl + 1) * 128], vb[:], al == 0, al == AG - 1,
                                       reads=[GB, vbB], writes=[banks[tl][1]])
                            for tl in range(NTC):
                                osl = oacc[:, tl, db * 512:(db + 1) * 512]
                                tt(osl, osl, banks[tl][0][:], ALU.add, reads=[banks[tl][1]], writes=[oaccB[tl]])
                    for tl in range(NTC):
                        row = c * TC + tl * 128
                        final_ops.append(dma(out[row:row + 128, :], oacc[:, tl, :], reads=[oaccB[tl]]))
            P.barrier()

        if not final_ops:
            final_ops.append(P.all_ops[-1])
        P.prepare(eng_sems, dma_sems, final_ops)
        block = es0.enter_context(nc.Block())
        for nm, en in (("sync", SP), ("tensor", PE), ("scalar", ACT), ("vector", DVE), ("gpsimd", POOL)):
            getattr(block, nm)(lambda e, en=en: P.emit(en, e))
    return nc


def rope_cs(S):
    rows = S // 64
    row = np.repeat(np.arange(rows, dtype=np.float32), 64)
    col = np.tile(np.arange(64, dtype=np.float32), rows)
    inv = (np.float32(10000.0) ** (-np.arange(32, dtype=np.float32) / np.float32(32))).astype(np.float32)
    ang = np.concatenate([row[:, None] * inv, col[:, None] * inv], axis=-1).astype(np.float32)
    return np.concatenate([np.cos(ang), np.sin(ang)], axis=-1).astype(np.float32)


def make_in_maps(inputs, S, NC):
    TOK = S // NC
    f = lambda a: np.ascontiguousarray(np.asarray(a, dtype=np.float32))
    x = f(inputs["x"]).reshape(-1, D)[:S]
    cs = rope_cs(S)
    shared = {
        "x": x, "cs": cs,
        "norm_mix": f(inputs["norm_mix"]).reshape(16, 128),
        "w_in": f(inputs["w_in"]).reshape(D, 7680),
        "b_gate": f(inputs["b_gate"]).reshape(32, 128),
        "q_norm": f(inputs["q_norm"]).reshape(1, 128),
        "k_norm": f(inputs["k_norm"]).reshape(1, 128),
        "sgu_norm": f(inputs["sgu_norm"]).reshape(1, 1024),
        "w_sgu": f(inputs["w_sgu"]).reshape(8, 128, 128),
        "b_sgu": f(inputs["b_sgu"]).reshape(1, 1024),
        "w_attn_proj": f(inputs["w_attn_proj"]).reshape(1024, D),
        "w_sgu_proj": f(inputs["w_sgu_proj"]).reshape(1024, D),
        "w_out": f(inputs["w_out"]).reshape(D, D),
        "norm_ffn": f(inputs["norm_ffn"]).reshape(16, 128),
        "w_peer_q": f(inputs["w_peer_q"]).reshape(D, D),
        "peer_k1": f(inputs["peer_k1"]).reshape(8, 128, 128),
        "peer_k2": f(inputs["peer_k2"]).reshape(8, 128, 128),
        "peer_u": f(inputs["peer_u"]).reshape(NEXP, D),
        "peer_v": f(inputs["peer_v"]).reshape(NEXP, D),
    }
    maps = []
    for c in range(NC):
        m = dict(shared)
        m["x_own"] = np.ascontiguousarray(x[c * TOK:(c + 1) * TOK])
        m["cs_own"] = np.ascontiguousarray(cs[c * TOK:(c + 1) * TOK])
        maps.append(m)
    return maps


_NC_CACHE = {}


def kernel(**inputs):
    S, NC = 16384, 8
    if "nc" not in _NC_CACHE:
        _NC_CACHE["nc"] = build(S, NC)
    nc = _NC_CACHE["nc"]
    in_maps = make_in_maps(inputs, S, NC)
    res = run_bass_kernel_spmd(nc, in_maps, core_ids=list(range(NC)))
    outs = [np.asarray(r["out"], dtype=np.float32) for r in res.results]
    return np.concatenate(outs, axis=0).reshape(1, S, D)
```
